# Optimizing a Trainium2 kernel written in Bass

```python
import math
import jax, jax.numpy as jnp
from jax import lax
import numpy as np

D_MODEL = 2048
BATCH = 1
SEQ = 16384
DEPTH = 1

EPS = 1e-6
SSM_D_INNER = 2 * D_MODEL
SSM_HEAD_DIM = 64
SSM_N_HEADS = SSM_D_INNER // SSM_HEAD_DIM
SSM_N_GROUPS = 8
SSM_HEADS_PER_GROUP = SSM_N_HEADS // SSM_N_GROUPS
SSM_D_STATE = 128
SSM_CONV = 4
SSM_CHUNK = 128
SSM_CONV_DIM = SSM_D_INNER + 2 * SSM_N_GROUPS * SSM_D_STATE
SSM_PROJ = SSM_D_INNER + SSM_CONV_DIM + SSM_N_HEADS
GDN_N_HEADS = 16
GDN_HEAD_K = 128
GDN_HEAD_V = 256
GDN_KEY_DIM = GDN_N_HEADS * GDN_HEAD_K
GDN_VAL_DIM = GDN_N_HEADS * GDN_HEAD_V
GDN_CONV = 4
GDN_CHUNK = 64
GDN_CONV_DIM = 2 * GDN_KEY_DIM + GDN_VAL_DIM
GDN_PROJ = GDN_CONV_DIM + GDN_VAL_DIM + 2 * GDN_N_HEADS
N_BRANCHES = 2
GATE_PROJ = N_BRANCHES * D_MODEL
IN_PROJ = SSM_PROJ + GDN_PROJ + GATE_PROJ
PEER_HEADS = 8
PEER_N_KEYS = 128
PEER_N_EXPERTS = PEER_N_KEYS * PEER_N_KEYS
PEER_D_KEY = 256
PEER_HALF = PEER_D_KEY // 2
PEER_TOPK = 16
PEER_TOKEN_BLOCK = 128

kernel_name = "hybrid_ssd_gdn_peer_block"


def _rms_norm(x, w):
    xf = x.astype(jnp.float32)
    y = xf * lax.rsqrt(jnp.mean(xf * xf, axis=-1, keepdims=True) + EPS)
    return (y * w.astype(jnp.float32)).astype(x.dtype)


def _l2norm(x):
    return x * lax.rsqrt(jnp.sum(x * x, axis=-1, keepdims=True) + EPS)


def _causal_dwconv(x, w):
    k_w, t = w.shape[0], x.shape[1]
    xp = jnp.pad(x, ((0, 0), (k_w - 1, 0), (0, 0)))
    y = xp[:, 0:t] * w[0]
    for k in range(1, k_w):
        y = y + xp[:, k:k + t] * w[k]
    return y


def _ssd_branch(p, conv_w, conv_b, dt_bias, a_log, d_skip, norm_w):
    f32 = jnp.float32
    b_, t, _ = p.shape
    L, G, R, P, N = SSM_CHUNK, SSM_N_GROUPS, SSM_HEADS_PER_GROUP, SSM_HEAD_DIM, SSM_D_STATE
    nc = t // L
    z, xbc, dt = jnp.split(p, [SSM_D_INNER, SSM_D_INNER + SSM_CONV_DIM], axis=-1)
    xbc = jax.nn.silu(_causal_dwconv(xbc, conv_w) + conv_b).astype(f32)
    xs, bm, cm = jnp.split(xbc, [SSM_D_INNER, SSM_D_INNER + G * N], axis=-1)
    x = xs.reshape(b_, nc, L, G, R, P)
    bm = bm.reshape(b_, nc, L, G, N)
    cm = cm.reshape(b_, nc, L, G, N)
    dt = jax.nn.softplus(dt.astype(f32) + dt_bias.astype(f32)).reshape(b_, nc, L, G, R)
    a = -jnp.exp(a_log.astype(f32)).reshape(G, R)
    a_cs = jnp.cumsum(dt * a, axis=2)
    xdt = x * dt[..., None]
    causal = jnp.tril(jnp.ones((L, L), dtype=bool))
    seg = a_cs[:, :, :, None] - a_cs[:, :, None, :]
    decay = jnp.exp(jnp.where(causal[:, :, None, None], seg, -jnp.inf))
    cb = jnp.einsum('bclgn,bcsgn->bclsg', cm, bm)
    scores = cb[..., None] * decay
    y_diag = jnp.einsum('bclsgr,bcsgrp->bclgrp', scores, xdt)
    to_end = jnp.exp(a_cs[:, :, -1:] - a_cs)
    states = jnp.einsum('bcsgn,bcsgrp->bcgrpn', bm, xdt * to_end[..., None])
    c_dec = jnp.exp(a_cs[:, :, -1])

    def step(hs, inp):
        s_c, d_c = inp
        return hs * d_c[..., None, None] + s_c, hs

    h0 = jnp.zeros((b_, G, R, P, N), f32)
    _, h_in = lax.scan(step, h0, (jnp.moveaxis(states, 1, 0), jnp.moveaxis(c_dec, 1, 0)))
    h_in = jnp.moveaxis(h_in, 0, 1)
    y_off = jnp.einsum('bclgn,bcgrpn->bclgrp', cm, h_in) * jnp.exp(a_cs)[..., None]
    y = y_diag + y_off + x * d_skip.astype(f32).reshape(G, R)[:, :, None]
    y = y.reshape(b_, t, SSM_D_INNER) * jax.nn.silu(z.astype(f32))
    yg = y.reshape(b_, t, G, SSM_D_INNER // G)
    yg = yg * lax.rsqrt(jnp.mean(yg * yg, axis=-1, keepdims=True) + EPS)
    y = yg.reshape(b_, t, SSM_D_INNER) * norm_w.astype(f32)
    return y.astype(p.dtype)


def _gdn_branch(p, conv_w, dt_bias, a_log, norm_w):
    f32 = jnp.float32
    b_, t, _ = p.shape
    H, DK, DV, C = GDN_N_HEADS, GDN_HEAD_K, GDN_HEAD_V, GDN_CHUNK
    nc = t // C
    qkv, z, bt, al = jnp.split(
        p, [GDN_CONV_DIM, GDN_CONV_DIM + GDN_VAL_DIM, GDN_CONV_DIM + GDN_VAL_DIM + H], axis=-1)
    qkv = jax.nn.silu(_causal_dwconv(qkv, conv_w)).astype(f32)
    q, k, v = jnp.split(qkv, [GDN_KEY_DIM, 2 * GDN_KEY_DIM], axis=-1)
    q = _l2norm(q.reshape(b_, t, H, DK)) * (DK ** -0.5)
    k = _l2norm(k.reshape(b_, t, H, DK))
    v = v.reshape(b_, t, H, DV)
    beta = jax.nn.sigmoid(bt.astype(f32))
    g = -jnp.exp(a_log.astype(f32)) * jax.nn.softplus(al.astype(f32) + dt_bias.astype(f32))

    def chunked(a):
        return jnp.moveaxis(a.reshape((b_, nc, C) + a.shape[2:]), 2, 3)

    q, k, v, beta, g = chunked(q), chunked(k), chunked(v), chunked(beta), chunked(g)
    gc = jnp.cumsum(g, axis=-1)
    causal = jnp.tril(jnp.ones((C, C), dtype=bool))
    strict = jnp.tril(jnp.ones((C, C), dtype=bool), k=-1)
    decay = jnp.exp(jnp.where(causal, gc[..., :, None] - gc[..., None, :], -jnp.inf))
    kk = jnp.einsum('bnhid,bnhjd->bnhij', k, k)
    lmat = jnp.where(strict, kk * beta[..., :, None] * decay, 0.0)
    eye = jnp.eye(C, dtype=f32)
    t_inv = lax.linalg.triangular_solve(eye + lmat, jnp.broadcast_to(eye, lmat.shape),
                                        left_side=True, lower=True, unit_diagonal=True)
    u = t_inv @ (v * beta[..., None])
    w = t_inv @ (k * (beta * jnp.exp(gc))[..., None])
    attn = jnp.where(causal, jnp.einsum('bnhid,bnhjd->bnhij', q, k) * decay, 0.0)
    q_dec = q * jnp.exp(gc)[..., None]
    k_dec = k * jnp.exp(gc[..., -1:] - gc)[..., None]
    c_dec = jnp.exp(gc[..., -1])

    def step(s, inp):
        u_c, w_c, a_c, q_c, k_c, d_c = inp
        v_new = u_c - w_c @ s
        o_c = q_c @ s + a_c @ v_new
        s = s * d_c[..., None, None] + jnp.swapaxes(k_c, -1, -2) @ v_new
        return s, o_c

    xs = (jnp.moveaxis(u, 1, 0), jnp.moveaxis(w, 1, 0), jnp.moveaxis(attn, 1, 0),
          jnp.moveaxis(q_dec, 1, 0), jnp.moveaxis(k_dec, 1, 0), jnp.moveaxis(c_dec, 1, 0))
    s0 = jnp.zeros((b_, H, DK, DV), f32)
    _, o = lax.scan(step, s0, xs)
    o = jnp.moveaxis(jnp.moveaxis(o, 0, 1), 2, 3).reshape(b_, t, H, DV)
    o = o * lax.rsqrt(jnp.mean(o * o, axis=-1, keepdims=True) + EPS) * norm_w.astype(f32)
    o = o * jax.nn.silu(z.astype(f32).reshape(b_, t, H, DV))
    return o.reshape(b_, t, GDN_VAL_DIM).astype(p.dtype)


def _peer_ffn(xn, w_q, sub_keys, u_tab, v_tab):
    f32 = jnp.float32
    b_, t, d = xn.shape
    q = (xn @ w_q).astype(f32).reshape(b_, t, PEER_HEADS, 2, PEER_HALF)
    s = jnp.einsum('bthpd,hpkd->bthpk', q, sub_keys.astype(f32))
    s1, i1 = lax.top_k(s[..., 0, :], PEER_TOPK)
    s2, i2 = lax.top_k(s[..., 1, :], PEER_TOPK)
    cand = (s1[..., :, None] + s2[..., None, :]).reshape(b_, t, PEER_HEADS, PEER_TOPK * PEER_TOPK)
    cand_idx = (i1[..., :, None] * PEER_N_KEYS + i2[..., None, :]).reshape(
        b_, t, PEER_HEADS, PEER_TOPK * PEER_TOPK)
    top_s, pos = lax.top_k(cand, PEER_TOPK)
    idx = jnp.take_along_axis(cand_idx, pos, axis=-1)
    gate = jax.nn.softmax(top_s, axis=-1).astype(xn.dtype)
    nb = (b_ * t) // PEER_TOKEN_BLOCK
    xb = xn.reshape(nb, PEER_TOKEN_BLOCK, d)
    idx_b = idx.reshape(nb, PEER_TOKEN_BLOCK, PEER_HEADS, PEER_TOPK)
    g_b = gate.reshape(nb, PEER_TOKEN_BLOCK, PEER_HEADS, PEER_TOPK)

    def block(args):
        xt, it, gt = args
        u = u_tab[it]
        act = jax.nn.gelu(jnp.einsum('thkd,td->thk', u, xt), approximate=False)
        return jnp.einsum('thk,thkd->td', gt * act, v_tab[it])

    out = lax.map(block, (xb, idx_b, g_b))
    return out.reshape(b_, t, d)


def setup_inputs(seed: int = 0) -> dict:
    key = jax.random.key(seed)
    ks = jax.random.split(key, 23)
    f32 = jnp.float32

    def nrm(k, shape, scale):
        return jax.random.normal(k, shape, f32) * scale

    def gain(k, shape):
        return 1.0 + 0.02 * jax.random.normal(k, shape, f32)

    def dt_bias(k, shape):
        dt = jnp.exp(jax.random.uniform(k, shape, f32, math.log(1e-3), math.log(1e-1)))
        return dt + jnp.log(-jnp.expm1(-dt))

    def a_log(k, shape):
        return jnp.log(jax.random.uniform(k, shape, f32, 1.0, 16.0))

    return {
        "x": jax.random.normal(ks[0], (BATCH, SEQ, D_MODEL), f32),
        "mix_norm_w": gain(ks[1], (DEPTH, D_MODEL)),
        "w_in": nrm(ks[2], (DEPTH, D_MODEL, IN_PROJ), D_MODEL ** -0.5),
        "gate_b": nrm(ks[3], (DEPTH, N_BRANCHES, D_MODEL), 0.02),
        "ssm_conv_w": nrm(ks[4], (DEPTH, SSM_CONV, SSM_CONV_DIM), SSM_CONV ** -0.5),
        "ssm_conv_b": nrm(ks[5], (DEPTH, SSM_CONV_DIM), 0.02),
        "ssm_dt_bias": dt_bias(ks[6], (DEPTH, SSM_N_HEADS)),
        "ssm_a_log": a_log(ks[7], (DEPTH, SSM_N_HEADS)),
        "ssm_d": 1.0 + 0.1 * jax.random.normal(ks[8], (DEPTH, SSM_N_HEADS), f32),
        "ssm_norm_w": gain(ks[9], (DEPTH, SSM_D_INNER)),
        "gdn_conv_w": nrm(ks[10], (DEPTH, GDN_CONV, GDN_CONV_DIM), GDN_CONV ** -0.5),
        "gdn_dt_bias": dt_bias(ks[11], (DEPTH, GDN_N_HEADS)),
        "gdn_a_log": a_log(ks[12], (DEPTH, GDN_N_HEADS)),
        "gdn_norm_w": gain(ks[13], (DEPTH, GDN_HEAD_V)),
        "w_branch_ssm": nrm(ks[14], (DEPTH, SSM_D_INNER, D_MODEL), SSM_D_INNER ** -0.5),
        "w_branch_gdn": nrm(ks[15], (DEPTH, GDN_VAL_DIM, D_MODEL), GDN_VAL_DIM ** -0.5),
        "w_out": nrm(ks[16], (DEPTH, D_MODEL, D_MODEL), D_MODEL ** -0.5),
        "ffn_norm_w": gain(ks[17], (DEPTH, D_MODEL)),
        "peer_w_q": nrm(ks[18], (DEPTH, D_MODEL, PEER_HEADS * PEER_D_KEY), D_MODEL ** -0.5),
        "peer_sub_keys": nrm(ks[19], (DEPTH, PEER_HEADS, 2, PEER_N_KEYS, PEER_HALF), PEER_HALF ** -0.5),
        "peer_u": nrm(ks[20], (DEPTH, PEER_N_EXPERTS, D_MODEL), D_MODEL ** -0.5),
        "peer_v": nrm(ks[21], (DEPTH, PEER_N_EXPERTS, D_MODEL), PEER_HEADS ** -0.5),
        "final_norm_w": gain(ks[22], (D_MODEL,)),
    }


def reference(x, mix_norm_w, w_in, gate_b, ssm_conv_w, ssm_conv_b, ssm_dt_bias, ssm_a_log,
              ssm_d, ssm_norm_w, gdn_conv_w, gdn_dt_bias, gdn_a_log, gdn_norm_w,
              w_branch_ssm, w_branch_gdn, w_out, ffn_norm_w, peer_w_q, peer_sub_keys,
              peer_u, peer_v, final_norm_w):
    b_, t, _ = x.shape
    h = x
    for l in range(DEPTH):
        xn = _rms_norm(h, mix_norm_w[l])
        proj = xn @ w_in[l]
        p_ssm, p_gdn, p_gate = jnp.split(proj, [SSM_PROJ, SSM_PROJ + GDN_PROJ], axis=-1)
        y_ssm = _ssd_branch(p_ssm, ssm_conv_w[l], ssm_conv_b[l], ssm_dt_bias[l], ssm_a_log[l],
                            ssm_d[l], ssm_norm_w[l]) @ w_branch_ssm[l]
        y_gdn = _gdn_branch(p_gdn, gdn_conv_w[l], gdn_dt_bias[l], gdn_a_log[l],
                            gdn_norm_w[l]) @ w_branch_gdn[l]
        gates = jax.nn.sigmoid(
            (p_gate.reshape(b_, t, N_BRANCHES, D_MODEL) + gate_b[l]).astype(jnp.float32)
        ).astype(x.dtype)
        mixed = gates[:, :, 0] * y_ssm + gates[:, :, 1] * y_gdn
        h = h + mixed @ w_out[l]
        hn = _rms_norm(h, ffn_norm_w[l])
        h = h + _peer_ffn(hn, peer_w_q[l], peer_sub_keys[l], peer_u[l], peer_v[l])
    return _rms_norm(h, final_norm_w)
```

```python
import numpy as np
from contextlib import ExitStack
import concourse.bass as bass
import concourse.mybir as mybir
from concourse.bass_utils import run_bass_kernel_spmd

F32 = mybir.dt.float32
BF16 = mybir.dt.bfloat16
U32 = mybir.dt.uint32
AF = mybir.ActivationFunctionType
ALU = mybir.AluOpType
AX = mybir.AxisListType

D_MODEL = 2048
SEQ = 16384
NCORES = 8
EPS = 1e-6
NEG = -30000.0
SSM_PROJ = 10304
GDN_PROJ = 12320


class Sched:
    NDMA = 12
    LIMIT = 30000

    def __init__(self, nc, es):
        self.nc = nc
        self.es = es
        self.E = dict(pe=nc.tensor, act=nc.scalar, dve=nc.vector, pool=nc.gpsimd, sp=nc.sync)
        self.sem = {}
        self.cnt = {}
        self.gen = {}
        for k in ['pe', 'act', 'dve', 'pool']:
            self._newsem(k)
        self.dq = {q: dict(sem=[None] * self.NDMA, use=[0] * self.NDMA, gen=[0] * self.NDMA, nxt=0) for q in ['sp', 'pool', 'act']}
        self.lastw = {}
        self.reads = {}
        self.waited = {}
        self.bank_last = {}
        self.nwaits = 0
        self.nops = 0

    def _newsem(self, k):
        g = self.gen.get(k, -1) + 1
        self.gen[k] = g
        self.sem[k] = self.es.enter_context(self.nc.semaphore(f"s_{k}{g}"))
        self.cnt[k] = 0

    def _wait(self, eng, ev):
        sem, val, sid = ev
        if self.waited.get((eng, sid), 0) >= val:
            return
        self.E[eng].wait_ge(sem, val)
        self.nwaits += 1
        self.waited[(eng, sid)] = val

    def _deps(self, eng, reads, writes):
        best = {}

        def add(e):
            if e is None:
                return
            if e[2] not in best or best[e[2]][1] < e[1]:
                best[e[2]] = e
        for k in list(reads) + list(writes):
            add(self.lastw.get(k))
        for k in writes:
            for e in self.reads.get(k, {}).values():
                add(e)
        for k in list(reads) + list(writes):
            if len(k) > 1 and k[0] == 'P' and k[1].isdigit():
                for sid, e in self.bank_last.get(k[1], {}).items():
                    if sid[0] != eng:
                        add(e)
        for e in best.values():
            if eng == 'pe' and e[2][0] == 'pe':
                continue
            self._wait(eng, e)

    def _record(self, ev, reads, writes):
        for k in list(reads) + list(writes):
            if len(k) > 1 and k[0] == 'P' and k[1].isdigit():
                self.bank_last.setdefault(k[1], {})[ev[2]] = ev
        for k in writes:
            self.lastw[k] = ev
            self.reads[k] = {}
        for k in reads:
            d = self.reads.setdefault(k, {})
            d[ev[2]] = ev

    def op(self, eng, fn, reads=(), writes=()):
        if self.cnt[eng] >= self.LIMIT:
            self._newsem(eng)
        self._deps(eng, reads, writes)
        ins = fn(self.E[eng])
        self.cnt[eng] += 1
        self.nops += 1
        ins.then_inc(self.sem[eng], 1)
        ev = (self.sem[eng], self.cnt[eng], (eng, self.gen[eng]))
        self._record(ev, reads, writes)
        return ev

    def dma(self, q, out, in_, reads=(), writes=(), **kw):
        D = self.dq[q]
        i = D['nxt']
        D['nxt'] = (i + 1) % self.NDMA
        if D['sem'][i] is None or 16 * (D['use'][i] + 1) > self.LIMIT:
            if D['sem'][i] is not None:
                self._wait(q, (D['sem'][i], 16 * D['use'][i], ('d', q, i, D['gen'][i])))
            D['gen'][i] += 1
            D['sem'][i] = self.es.enter_context(self.nc.semaphore(f"d{q}_{i}_{D['gen'][i]}"))
            D['use'][i] = 0
        sid = ('d', q, i, D['gen'][i])
        if D['use'][i] > 0:
            self._wait(q, (D['sem'][i], 16 * D['use'][i], sid))
        self._deps(q, reads, writes)
        D['use'][i] += 1
        ins = self.E[q].dma_start(out=out, in_=in_, **kw)
        ins.then_inc(D['sem'][i], 16)
        ev = (D['sem'][i], 16 * D['use'][i], sid)
        self._record(ev, reads, writes)
        return ev

    def barrier(self):
        for eng in ['pe', 'act', 'dve', 'pool', 'sp']:
            for other in ['pe', 'act', 'dve', 'pool']:
                if other != eng and self.cnt[other] > 0:
                    self._wait(eng, (self.sem[other], self.cnt[other], (other, self.gen[other])))
            for q, D in self.dq.items():
                for i in range(self.NDMA):
                    if D['sem'][i] is not None and D['use'][i] > 0:
                        self._wait(eng, (D['sem'][i], 16 * D['use'][i], ('d', q, i, D['gen'][i])))

    def wait_all(self, eng):
        best = {}
        for e in self.lastw.values():
            if e[2] not in best or best[e[2]][1] < e[1]:
                best[e[2]] = e
        for e in best.values():
            self._wait(eng, e)


def bc(ap, axis, n):
    dims = [list(d) for d in ap.ap]
    dims.insert(axis, [0, n])
    return bass.AP(ap.tensor, ap.offset, dims)


def kq(b):
    return [f'P{b}q{j}' for j in range(4)]


class Ctx:
    def __init__(self, nc, es):
        self.nc = nc
        self.es = es
        self.S = Sched(nc, es)
        self.scopes = []
        self.uid = 0

    def sb(self, name, shape, dt):
        es = self.scopes[-1] if self.scopes else self.es
        if self.scopes:
            self.uid += 1
            name = f"{name}_u{self.uid}"
        return es.enter_context(self.nc.sbuf_tensor(name, list(shape), dt))

    def push(self):
        sc = ExitStack()
        self.scopes.append(sc)
        return sc

    def pop(self):
        self.S.barrier()
        self.scopes.pop().close()

    def ps(self, name, shape=(128, 512), dt=F32):
        es = self.scopes[-1] if self.scopes else self.es
        if self.scopes:
            self.uid += 1
            name = f"{name}_u{self.uid}"
        return es.enter_context(self.nc.psum_tensor(name, list(shape), dt))


def build_consts(C):
    S, nc = C.S, C.nc
    K = {}
    ones_f = C.sb("ones_f", [128, 128], F32)
    zero_f = C.sb("zero_f", [128, 128], F32)
    S.op('pool', lambda e: e.memset(ones_f[:], 1.0), writes=['ones_f'])
    S.op('pool', lambda e: e.memset(zero_f[:], 0.0), writes=['zero_f'])

    def sel(name, src, srckey, base, cm, step, fill):
        t = C.sb(name, [128, 128], F32)
        S.op('pool', lambda e: e.affine_select(out=t[:], in_=src[:], pattern=[[step, 128]],
                                               compare_op=ALU.is_ge, fill=fill, base=base, channel_multiplier=cm),
             reads=[srckey], writes=[name])
        return t
    tmp = sel("tmp_ge", ones_f, 'ones_f', 0, -1, 1, 0.0)
    ident_f = sel("ident_f", tmp, 'tmp_ge', 0, 1, -1, 0.0)
    tri_f = tmp
    negmask = sel("negmask", zero_f, 'zero_f', 0, -1, 1, NEG)
    negmask_s = sel("negmask_s", zero_f, 'zero_f', -1, -1, 1, NEG)
    tribd = C.sb("tribd", [128, 128], F32)
    S.op('pool', lambda e: e.tensor_copy(out=tribd[:], in_=tri_f[:]), reads=['tmp_ge'], writes=['tribd'])
    S.op('pool', lambda e: e.memset(tribd[0:64, 64:128], 0.0), writes=['tribd'])
    nm_bd = C.sb("nm_bd", [128, 128], F32)
    S.op('pool', lambda e: e.tensor_copy(out=nm_bd[:], in_=negmask[:]), reads=['negmask'], writes=['nm_bd'])
    S.op('pool', lambda e: e.memset(nm_bd[0:64, 64:128], NEG), writes=['nm_bd'])
    nm_bds = C.sb("nm_bds", [128, 128], F32)
    S.op('pool', lambda e: e.tensor_copy(out=nm_bds[:], in_=negmask_s[:]), reads=['negmask_s'], writes=['nm_bds'])
    S.op('pool', lambda e: e.memset(nm_bds[0:64, 64:128], NEG), writes=['nm_bds'])
    selA = C.sb("selA", [128, 128], F32)
    selB = C.sb("selB", [128, 128], F32)
    blk1 = C.sb("blk1", [128, 128], F32)
    S.op('pool', lambda e: e.memset(selA[:], 0.0), writes=['selA'])
    S.op('pool', lambda e: e.memset(selA[0:64, :], 1.0), writes=['selA'])
    S.op('pool', lambda e: e.memset(selB[:], 0.0), writes=['selB'])
    S.op('pool', lambda e: e.memset(selB[64:128, :], 1.0), writes=['selB'])
    S.op('pool', lambda e: e.memset(blk1[:], 0.0), writes=['blk1'])
    S.op('pool', lambda e: e.memset(blk1[0:64, 0:64], 1.0), writes=['blk1'])
    S.op('pool', lambda e: e.memset(blk1[64:128, 64:128], 1.0), writes=['blk1'])
    ident_b = C.sb("ident_b", [128, 128], BF16)
    ones_b = C.sb("ones_b", [128, 128], BF16)
    S.op('pool', lambda e: e.tensor_copy(out=ident_b[:], in_=ident_f[:]), reads=['ident_f'], writes=['ident_b'])
    S.op('pool', lambda e: e.tensor_copy(out=ones_b[:], in_=ones_f[:]), reads=['ones_f'], writes=['ones_b'])
    eps_t = C.sb("eps_t", [128, 1], F32)
    one_t = C.sb("one_t", [128, 1], F32)
    S.op('pool', lambda e: e.memset(eps_t[:], EPS), writes=['eps_t'])
    S.op('pool', lambda e: e.memset(one_t[:], 1.0), writes=['one_t'])
    K.update(eps_t=eps_t, one_t=one_t)
    K.update(ones_f=ones_f, zero_f=zero_f, ident_f=ident_f, tri_f=tri_f, negmask=negmask, negmask_s=negmask_s,
             tribd=tribd, nm_bd=nm_bd, nm_bds=nm_bds, selA=selA, selB=selB, blk1=blk1, ident_b=ident_b, ones_b=ones_b)
    return K


class QAlloc:
    def __init__(self, P, banks):
        self.P = P
        self.banks = banks
        self.i = [0 for _ in banks]

    def _next(self, hh):
        b = self.banks[hh][self.i[hh] % len(self.banks[hh])]
        self.i[hh] += 1
        return b

    def Q(self, hh=0):
        b = self._next(hh)
        return self.P[b][:, 0:128], [f'P{b}q0']

    def H(self, hh=0):
        b = self._next(hh)
        return self.P[b][:, 0:256], [f'P{b}q0', f'P{b}q1']


def gdn_pair(C, K, P, QA, so, ys, ysk, sub, g_t, beta_t, lnb_t, Sg_f, Sg_b, gsq, rn, kn, qn, qd, kb, kdec, vb, gsc,
             Xg, Xg2, Eg, Ee, Es, attnT, Mm, Lm, Tm, Ttm, Pm, Ptm, Tb, nwT, vnb, dcb):
    S = C.S
    ones_f, ident_f, ones_b, ident_b = K['ones_f'], K['ident_f'], K['ones_b'], K['ident_b']
    cs = slice(sub * 128, (sub + 1) * 128)
    HH = range(2)
    evi = [0]

    def evac(out, in_, reads, writes):
        evi[0] += 1
        if evi[0] % 2:
            S.op('act', lambda e: e.copy(out=out, in_=in_), reads=reads, writes=writes)
        else:
            S.op('dve', lambda e: e.tensor_copy(out=out, in_=in_), reads=reads, writes=writes)

    for hh in HH:
        S.op('act', lambda e, hh=hh: e.activation(out=gsq[hh][:, 0, :], in_=so[:, 8 + hh, cs], func=AF.Square),
             reads=[f'so{8 + hh}'], writes=[f'gsq{hh}'])
        S.op('act', lambda e, hh=hh: e.activation(out=gsq[hh][:, 1, :], in_=so[:, 6 + hh, cs], func=AF.Square),
             reads=[f'so{6 + hh}'], writes=[f'gsq{hh}'])
        hq_, hk = QA.H(hh)
        S.op('pe', lambda e, hh=hh, hq_=hq_: e.matmul(hq_, lhsT=ones_b[:], rhs=gsq[hh][:].rearrange("p a t -> p (a t)"), start=True, stop=True),
             reads=['ones_b', f'gsq{hh}'], writes=hk)
        S.op('act', lambda e, hh=hh, hq_=hq_: e.activation(out=rn[hh][:].rearrange("p a t -> p (a t)"), in_=hq_, func=AF.Ln, bias=K['eps_t'][:]),
             reads=hk + ['eps_t'], writes=[f'rn{hh}'])
        S.op('act', lambda e, hh=hh: e.activation(out=rn[hh][:], in_=rn[hh][:], func=AF.Exp, scale=-0.5), reads=[f'rn{hh}'], writes=[f'rn{hh}'])
        S.op('dve', lambda e, hh=hh: e.tensor_tensor(out=kn[hh][:], in0=so[:, 8 + hh, cs], in1=rn[hh][:, 0, :], op=ALU.mult),
             reads=[f'so{8 + hh}', f'rn{hh}'], writes=[f'kn{hh}'])
        S.op('dve', lambda e, hh=hh: e.scalar_tensor_tensor(out=qn[hh][:], in0=so[:, 6 + hh, cs], scalar=128.0 ** -0.5, in1=rn[hh][:, 1, :],
                                                           op0=ALU.mult, op1=ALU.mult),
             reads=[f'so{6 + hh}', f'rn{hh}'], writes=[f'qn{hh}'])
    q7, q7k = QA.Q(0)
    S.op('pe', lambda e: e.matmul(q7[:, 0:2], lhsT=K['tribd'][:], rhs=g_t[:, sub, :], start=True, stop=True), reads=['tribd', 'g_t'], writes=q7k)
    S.op('pe', lambda e: e.matmul(q7[:, 2:4], lhsT=K['blk1'][:], rhs=g_t[:, sub, :], start=True, stop=True), reads=['blk1', 'g_t'], writes=q7k)
    S.op('pe', lambda e: e.matmul(q7[:, 4:6], lhsT=K['selA'][:], rhs=g_t[:, sub, :], start=True, stop=True), reads=['selA', 'g_t'], writes=q7k)
    S.op('pe', lambda e: e.matmul(q7[:, 6:8], lhsT=K['selB'][:], rhs=g_t[:, sub, :], start=True, stop=True), reads=['selB', 'g_t'], writes=q7k)
    for hh in HH:
        gk = f'gsc{hh}'
        S.op('dve', lambda e, hh=hh: e.tensor_copy(out=gsc[hh][:, 0:1], in_=q7[:, hh:hh + 1]), reads=q7k, writes=[gk])
        S.op('dve', lambda e, hh=hh: e.tensor_scalar(out=gsc[hh][:, 1:2], in0=q7[:, hh:hh + 1], scalar1=-1.0, scalar2=None, op0=ALU.mult),
             reads=q7k, writes=[gk])
        S.op('dve', lambda e, hh=hh: e.tensor_tensor(out=gsc[hh][:, 4:5], in0=q7[:, 2 + hh:3 + hh], in1=q7[:, hh:hh + 1], op=ALU.subtract) if False else
             e.tensor_tensor(out=gsc[hh][:, 4:5], in0=q7[:, 2 + hh:3 + hh], in1=gsc[hh][:, 0:1], op=ALU.subtract),
             reads=q7k + [gk], writes=[gk])
        S.op('act', lambda e, hh=hh: e.activation(out=gsc[hh][:, 4:5], in_=gsc[hh][:, 4:5], func=AF.Exp), reads=[gk], writes=[gk])
        S.op('act', lambda e, hh=hh: e.activation(out=gsc[hh][:, 3:4], in_=gsc[hh][:, 0:1], func=AF.Exp), reads=[gk], writes=[gk])
        S.op('dve', lambda e, hh=hh: e.tensor_tensor(out=gsc[hh][:, 3:4], in0=gsc[hh][:, 3:4], in1=beta_t[:, sub, hh:hh + 1], op=ALU.mult),
             reads=[gk, 'beta_t'], writes=[gk])
        S.op('act', lambda e, hh=hh: e.activation(out=dcb[hh][:, 0:1], in_=q7[:, 4 + hh:5 + hh], func=AF.Exp), reads=q7k, writes=[f'dcb{hh}'])
        S.op('act', lambda e, hh=hh: e.activation(out=dcb[hh][:, 1:2], in_=q7[:, 6 + hh:7 + hh], func=AF.Exp), reads=q7k, writes=[f'dcb{hh}'])
    for hh in HH:
        qk_, qkk = QA.Q(hh)
        S.op('pe', lambda e, hh=hh, qk_=qk_: e.matmul(qk_, lhsT=kn[hh][:], rhs=ident_b[:], start=True, stop=True),
             reads=[f'kn{hh}', 'ident_b'], writes=qkk)
        S.op('dve', lambda e, hh=hh, qk_=qk_: e.tensor_scalar(out=kb[hh][:], in0=qk_, scalar1=gsc[hh][:, 3:4], scalar2=None, op0=ALU.mult),
             reads=qkk + [f'gsc{hh}'], writes=[f'kb{hh}'])
        S.op('dve', lambda e, hh=hh, qk_=qk_: e.tensor_scalar(out=kdec[hh][:], in0=qk_, scalar1=gsc[hh][:, 4:5], scalar2=None, op0=ALU.mult),
             reads=qkk + [f'gsc{hh}'], writes=[f'kdec{hh}'])
        hv, hvk = QA.H(hh)
        for vbk in range(2):
            S.op('pe', lambda e, hh=hh, vbk=vbk, hv=hv: e.transpose(hv[:, vbk * 128:(vbk + 1) * 128], so[:, 10 + 2 * hh + vbk, cs], ident_f[:]),
                 reads=[f'so{10 + 2 * hh + vbk}', 'ident_f'], writes=[hvk[vbk]])
        S.op('dve', lambda e, hh=hh, hv=hv: e.tensor_scalar(out=vb[hh][:], in0=hv, scalar1=beta_t[:, sub, hh:hh + 1], scalar2=None, op0=ALU.mult),
             reads=hvk + ['beta_t'], writes=[f'vb{hh}'])
    for hh in HH:
        S.op('dve', lambda e, hh=hh: e.tensor_scalar(out=Xg[hh][:], in0=K['tribd'][:], scalar1=g_t[:, sub, hh:hh + 1], scalar2=None, op0=ALU.mult),
             reads=['tribd', 'g_t'], writes=[f'Xg{hh}'])
        S.op('dve', lambda e, hh=hh: e.scalar_tensor_tensor(out=Xg2[hh][:], in0=ident_f[:], scalar=lnb_t[:, sub, hh:hh + 1], in1=Xg[hh][:],
                                                           op0=ALU.mult, op1=ALU.add),
             reads=['ident_f', 'lnb_t', f'Xg{hh}'], writes=[f'Xg2{hh}'])
        a, ak = QA.Q(hh)
        S.op('pe', lambda e, hh=hh, a=a: e.matmul(a, lhsT=ones_f[:], rhs=Xg[hh][:], start=True, stop=False), reads=['ones_f', f'Xg{hh}'], writes=ak)
        S.op('pe', lambda e, hh=hh, a=a: e.matmul(a, lhsT=ident_f[:], rhs=K['nm_bd'][:], start=False, stop=True), reads=['ident_f', 'nm_bd'], writes=ak)
        S.op('act', lambda e, hh=hh, a=a: e.activation(out=Ee[hh][:], in_=a, func=AF.Exp, bias=gsc[hh][:, 1:2]), reads=ak + [f'gsc{hh}'], writes=[f'Ee{hh}'])
        b_, bk = QA.Q(hh)
        S.op('pe', lambda e, hh=hh, b_=b_: e.matmul(b_, lhsT=ones_f[:], rhs=Xg2[hh][:], start=True, stop=False), reads=['ones_f', f'Xg2{hh}'], writes=bk)
        S.op('pe', lambda e, hh=hh, b_=b_: e.matmul(b_, lhsT=ident_f[:], rhs=K['nm_bds'][:], start=False, stop=True), reads=['ident_f', 'nm_bds'], writes=bk)
        S.op('act', lambda e, hh=hh, b_=b_: e.activation(out=Es[hh][:], in_=b_, func=AF.Exp, bias=gsc[hh][:, 1:2]), reads=bk + [f'gsc{hh}'], writes=[f'Es{hh}'])
        c_, ck = QA.Q(hh)
        S.op('pe', lambda e, hh=hh, c_=c_: e.matmul(c_, lhsT=ones_f[:], rhs=Xg[hh][:], start=True, stop=True), reads=['ones_f', f'Xg{hh}'], writes=ck)
        S.op('act', lambda e, hh=hh, c_=c_: e.activation(out=Eg[hh][:], in_=c_, func=AF.Exp), reads=ck, writes=[f'Eg{hh}'])
        S.op('dve', lambda e, hh=hh: e.tensor_tensor(out=qd[hh][:], in0=qn[hh][:], in1=Eg[hh][:], op=ALU.mult),
             reads=[f'qn{hh}', f'Eg{hh}'], writes=[f'qd{hh}'])
        d_, dk_ = QA.Q(hh)
        S.op('pe', lambda e, hh=hh, d_=d_: e.matmul(d_, lhsT=kn[hh][:], rhs=kn[hh][:], start=True, stop=True), reads=[f'kn{hh}'], writes=dk_)
        S.op('dve', lambda e, hh=hh, d_=d_: e.tensor_tensor(out=Mm[hh][:], in0=d_, in1=Es[hh][:], op=ALU.mult), reads=dk_ + [f'Es{hh}'], writes=[f'Mm{hh}'])
        e_, ek = QA.Q(hh)
        S.op('pe', lambda e, hh=hh, e_=e_: e.matmul(e_, lhsT=kn[hh][:], rhs=qn[hh][:], start=True, stop=True), reads=[f'kn{hh}', f'qn{hh}'], writes=ek)
        S.op('dve', lambda e, hh=hh, e_=e_: e.tensor_tensor(out=attnT[hh][:], in0=e_, in1=Ee[hh][:], op=ALU.mult), reads=ek + [f'Ee{hh}'], writes=[f'attnT{hh}'])
    for hh in HH:
        a, ak = QA.Q(hh)
        S.op('pe', lambda e, hh=hh, a=a: e.transpose(a, Mm[hh][:], ident_f[:]), reads=[f'Mm{hh}', 'ident_f'], writes=ak)
        evac(Lm[hh][:], a, ak, [f'Lm{hh}'])
        S.op('dve', lambda e, hh=hh: e.tensor_tensor(out=Tm[hh][0][:], in0=ident_f[:], in1=Mm[hh][:], op=ALU.subtract),
             reads=['ident_f', f'Mm{hh}'], writes=[f'Tm{hh}_0'])
        S.op('pool', lambda e, hh=hh: e.tensor_tensor(out=Ttm[hh][0][:], in0=ident_f[:], in1=Lm[hh][:], op=ALU.subtract),
             reads=['ident_f', f'Lm{hh}'], writes=[f'Ttm{hh}_0'])
        a, ak = QA.Q(hh)
        S.op('pe', lambda e, hh=hh, a=a: e.matmul(a, lhsT=Lm[hh][:], rhs=Mm[hh][:], start=True, stop=True), reads=[f'Lm{hh}', f'Mm{hh}'], writes=ak)
        evac(Pm[hh][0][:], a, ak, [f'Pm{hh}_0'])
        a, ak = QA.Q(hh)
        S.op('pe', lambda e, hh=hh, a=a: e.matmul(a, lhsT=Mm[hh][:], rhs=Lm[hh][:], start=True, stop=True), reads=[f'Lm{hh}', f'Mm{hh}'], writes=ak)
        evac(Ptm[hh][0][:], a, ak, [f'Ptm{hh}_0'])
    for k in range(1, 6):
        cur, nxt = (k - 1) % 2, k % 2
        pc, pn = (k - 1) % 2, k % 2
        for hh in HH:
            a, ak = QA.Q(hh)
            S.op('pe', lambda e, hh=hh, a=a: e.matmul(a, lhsT=ident_f[:], rhs=Tm[hh][cur][:], start=True, stop=False),
                 reads=['ident_f', f'Tm{hh}_{cur}'], writes=ak)
            S.op('pe', lambda e, hh=hh, a=a: e.matmul(a, lhsT=Ttm[hh][cur][:], rhs=Pm[hh][pc][:], start=False, stop=True),
                 reads=[f'Ttm{hh}_{cur}', f'Pm{hh}_{pc}'], writes=ak)
            evac(Tm[hh][nxt][:], a, ak, [f'Tm{hh}_{nxt}'])
            if k < 5:
                a, ak = QA.Q(hh)
                S.op('pe', lambda e, hh=hh, a=a: e.matmul(a, lhsT=ident_f[:], rhs=Ttm[hh][cur][:], start=True, stop=False),
                     reads=['ident_f', f'Ttm{hh}_{cur}'], writes=ak)
                S.op('pe', lambda e, hh=hh, a=a: e.matmul(a, lhsT=Pm[hh][pc][:], rhs=Ttm[hh][cur][:], start=False, stop=True),
                     reads=[f'Ttm{hh}_{cur}', f'Pm{hh}_{pc}'], writes=ak)
                evac(Ttm[hh][nxt][:], a, ak, [f'Ttm{hh}_{nxt}'])
                a, ak = QA.Q(hh)
                S.op('pe', lambda e, hh=hh, a=a: e.matmul(a, lhsT=Ptm[hh][pc][:], rhs=Pm[hh][pc][:], start=True, stop=True),
                     reads=[f'Ptm{hh}_{pc}', f'Pm{hh}_{pc}'], writes=ak)
                evac(Pm[hh][pn][:], a, ak, [f'Pm{hh}_{pn}'])
                a, ak = QA.Q(hh)
                S.op('pe', lambda e, hh=hh, a=a: e.matmul(a, lhsT=Pm[hh][pc][:], rhs=Ptm[hh][pc][:], start=True, stop=True),
                     reads=[f'Ptm{hh}_{pc}', f'Pm{hh}_{pc}'], writes=ak)
                evac(Ptm[hh][pn][:], a, ak, [f'Ptm{hh}_{pn}'])
    for hh in HH:
        S.op('act', lambda e, hh=hh: e.copy(out=Tb[hh][:], in_=Tm[hh][1][:]), reads=[f'Tm{hh}_1'], writes=[f'Tb{hh}'])
        a, ak = QA.Q(hh)
        S.op('pe', lambda e, hh=hh, a=a: e.matmul(a, lhsT=kb[hh][:], rhs=Tb[hh][:], start=True, stop=True), reads=[f'kb{hh}', f'Tb{hh}'], writes=ak)
        S.op('act', lambda e, hh=hh, a=a: e.mul(out=nwT[hh][:], in_=a, mul=-1.0), reads=ak, writes=[f'nwT{hh}'])
    for c in range(2):
        jr = slice(c * 64, (c + 1) * 64)
        for hh in HH:
            hv, hvk = QA.H(hh)
            S.op('pe', lambda e, hh=hh, hv=hv: e.matmul(hv[jr, :], lhsT=Tb[hh][jr, jr], rhs=vb[hh][jr, :], start=True, stop=False),
                 reads=[f'Tb{hh}', f'vb{hh}'], writes=hvk)
            S.op('pe', lambda e, hh=hh, hv=hv: e.matmul(hv[jr, :], lhsT=nwT[hh][:, jr], rhs=Sg_b[hh][:], start=False, stop=True),
                 reads=[f'nwT{hh}', f'Sg_b{hh}'], writes=hvk)
            evac(vnb[hh][jr, :], hv[jr, :], hvk, [f'vnb{hh}'])
        for hh in HH:
            for dvb in range(2):
                a, ak = QA.Q(hh)
                S.op('pe', lambda e, hh=hh, dvb=dvb, a=a: e.matmul(a[:, 0:64], lhsT=Sg_b[hh][:, dvb * 128:(dvb + 1) * 128], rhs=qd[hh][:, jr],
                                                                 start=True, stop=False), reads=[f'Sg_b{hh}', f'qd{hh}'], writes=ak)
                S.op('pe', lambda e, hh=hh, dvb=dvb, a=a: e.matmul(a[:, 0:64], lhsT=vnb[hh][jr, dvb * 128:(dvb + 1) * 128], rhs=attnT[hh][jr, jr],
                                                                 start=False, stop=True), reads=[f'vnb{hh}', f'attnT{hh}'], writes=ak)
                evac(ys[:, 4 + 2 * hh + dvb, sub * 128 + c * 64:sub * 128 + c * 64 + 64], a[:, 0:64], ak, [ysk])
        for hh in HH:
            hv, hvk = QA.H(hh)
            S.op('pe', lambda e, hh=hh, hv=hv: e.matmul(hv, lhsT=kdec[hh][jr, :], rhs=vnb[hh][jr, :], start=True, stop=True),
                 reads=[f'kdec{hh}', f'vnb{hh}'], writes=hvk)
            S.op('dve', lambda e, hh=hh, hv=hv: e.scalar_tensor_tensor(out=Sg_f[hh][:], in0=Sg_f[hh][:], scalar=dcb[hh][:, c:c + 1], in1=hv,
                                                                     op0=ALU.mult, op1=ALU.add),
                 reads=[f'Sg_f{hh}', f'dcb{hh}'] + hvk, writes=[f'Sg_f{hh}'])
            S.op('act', lambda e, hh=hh: e.copy(out=Sg_b[hh][:], in_=Sg_f[hh][:]), reads=[f'Sg_f{hh}'], writes=[f'Sg_b{hh}'])

NF = 14
NCOL1 = NF * 128 + 16
TT = 256


def p1_decl(nc, T):
    return dict(
        xT=nc.dram_tensor("xT", [D_MODEL, T], F32, kind="ExternalInput").ap(),
        w1=nc.dram_tensor("w1", [D_MODEL, NCOL1], F32, kind="ExternalInput").ap(),
        mixw=nc.dram_tensor("mixw", [128, 16], F32, kind="ExternalInput").ap(),
        convw=nc.dram_tensor("convw", [128, NF, 4], F32, kind="ExternalInput").ap(),
        convb=nc.dram_tensor("convb", [128, NF], F32, kind="ExternalInput").ap(),
        hp=nc.dram_tensor("hp", [128, 32], F32, kind="ExternalInput").ap())


def p1_body(C, K, T, D, store):
    S = C.S
    NTT = T // TT
    xT_d, w1_d, mixw_d, convw_d, convb_d, hp_d = D['xT'], D['w1'], D['mixw'], D['convw'], D['convb'], D['hp']
    ones_f, ident_f, tri_f = K['ones_f'], K['ident_f'], K['tri_f']
    ones_b, ident_b = K['ones_b'], K['ident_b']
    W = C.sb("W", [128, 16, NCOL1], BF16)
    mixw = C.sb("mixw_s", [128, 16], F32)
    convw = C.sb("convw_s", [128, NF, 4], F32)
    convb = C.sb("convb_s", [128, NF], F32)
    hp = C.sb("hp_s", [128, 32], F32)
    hq = C.sb("hq", [128, 16], F32)
    S.dma('sp', mixw[:], mixw_d, writes=['mixw'])
    S.dma('sp', convw[:], convw_d, writes=['convw'])
    S.dma('sp', convb[:], convb_d, writes=['convb'])
    S.dma('sp', hp[:], hp_d, writes=['hp'])
    S.op('act', lambda e: e.activation(out=hq[:, 0:8], in_=hp[:, 8:16], func=AF.Exp), reads=['hp'], writes=['hq'])
    S.op('act', lambda e: e.activation(out=hq[:, 8:10], in_=hp[:, 22:24], func=AF.Exp), reads=['hp'], writes=['hq'])
    S.op('dve', lambda e: e.tensor_scalar(out=hq[:, 0:10], in0=hq[:, 0:10], scalar1=-1.0, scalar2=None, op0=ALU.mult),
         reads=['hq'], writes=['hq'])
    w1v = w1_d.rearrange("(k p) c -> p k c", p=128)
    for k in range(16):
        S.dma('pool', W[:, k, :], w1v[:, k, :], writes=[f'W{k}'])
    xs = [C.sb(f"xs{i}", [128, 4, TT], F32) for i in range(2)]
    xsq = [C.sb(f"xsq{i}", [128, 4, TT], BF16) for i in range(2)]
    xn = [C.sb(f"xn{i}", [128, 16, TT], BF16) for i in range(2)]
    rb = C.sb("rb", [128, TT], F32)
    convin = C.sb("convin", [128, NF, 3 + TT], F32)
    cacc = [C.sb(f"cacc{i}", [128, TT], F32) for i in range(2)]
    so = C.sb("so", [128, NF, TT], F32)
    smf = C.sb("smf", [16, TT], F32)
    smt = C.sb("smt", [128, 2, 16], F32)
    ystage = [C.sb(f"ystage{i}", [128, 8, TT], BF16) for i in range(2)]
    S.op('pool', lambda e: e.memset(convin[:], 0.0), writes=[f'convin{b}' for b in range(NF)])
    dt_t = C.sb("dt_t", [128, 2, 8], F32)
    dA_t = C.sb("dA_t", [128, 2, 8], F32)
    g_t = C.sb("g_t", [128, 2, 2], F32)
    beta_t = C.sb("beta_t", [128, 2, 2], F32)
    lnb_t = C.sb("lnb_t", [128, 2, 2], F32)
    sm_tmp = C.sb("sm_tmp", [128, 2, 16], F32)
    h_f = C.sb("h_f", [128, 8, 64], F32)
    h_b = C.sb("h_b", [128, 512], BF16)
    S.op('pool', lambda e: e.memset(h_f[:], 0.0), writes=['h_f'])
    S.op('pool', lambda e: e.memset(h_b[:], 0.0), writes=['h_b'])
    xtok = C.sb("xtok", [128, 8, 64], F32)
    xdt = C.sb("xdt", [128, 8, 64], BF16)
    xdtw = C.sb("xdtw", [128, 8, 64], BF16)
    btok = C.sb("btok", [128, 128], BF16)
    bfb = C.sb("bfb", [128, 128], BF16)
    cfb = C.sb("cfb", [128, 128], BF16)
    Xda = C.sb("Xda", [128, 8, 128], F32)
    acs = C.sb("acs", [128, 8], F32)
    nacs = C.sb("nacs", [128, 8], F32)
    tend = C.sb("tend", [128, 8], F32)
    cdec = C.sb("cdec", [128, 8], F32)
    wsc = C.sb("wsc", [128, 8], F32)
    decay = C.sb("decay", [128, 8, 128], BF16)
    Eb = C.sb("Eb", [128, 8, 128], F32)
    Cdec = C.sb("Cdec", [128, 8, 128], BF16)
    scor = C.sb("scor", [128, 8, 128], BF16)
    nmrep = C.sb("nmrep", [128, 8, 128], F32)
    for r in range(8):
        S.op('pool', lambda e, r=r: e.tensor_copy(out=nmrep[:, r, :], in_=K['negmask'][:]), reads=['negmask'], writes=['nmrep'])
    Sg_f = [C.sb(f"Sg_f{h}", [128, 256], F32) for h in range(2)]
    Sg_b = [C.sb(f"Sg_b{h}", [128, 256], BF16) for h in range(2)]
    for h in range(2):
        S.op('pool', lambda e, h=h: e.memset(Sg_f[h][:], 0.0), writes=[f'Sg_f{h}'])
        S.op('pool', lambda e, h=h: e.memset(Sg_b[h][:], 0.0), writes=[f'Sg_b{h}'])
    gsq = [C.sb(f"gsq{h}", [128, 2, 128], BF16) for h in range(2)]
    rn = [C.sb(f"rn{h}", [128, 2, 128], F32) for h in range(2)]
    kn = [C.sb(f"kn{h}", [128, 128], BF16) for h in range(2)]
    qn = [C.sb(f"qn{h}", [128, 128], BF16) for h in range(2)]
    qd = [C.sb(f"qd{h}", [128, 128], BF16) for h in range(2)]
    kb = [C.sb(f"kb{h}", [128, 128], BF16) for h in range(2)]
    kdec = [C.sb(f"kdec{h}", [128, 128], BF16) for h in range(2)]
    vb = [C.sb(f"vb{h}", [128, 256], BF16) for h in range(2)]
    gsc = [C.sb(f"gsc{h}", [128, 12], F32) for h in range(2)]
    Xg = [C.sb(f"Xg{h}", [128, 128], F32) for h in range(2)]
    Xg2 = [C.sb(f"Xg2{h}", [128, 128], F32) for h in range(2)]
    Eg = [C.sb(f"Eg{h}", [128, 128], F32) for h in range(2)]
    Ee = [C.sb(f"Ee{h}", [128, 128], F32) for h in range(2)]
    Es = [C.sb(f"Es{h}", [128, 128], F32) for h in range(2)]
    attnT = [C.sb(f"attnT{h}", [128, 128], BF16) for h in range(2)]
    Mm = [C.sb(f"Mm{h}", [128, 128], F32) for h in range(2)]
    Lm = [C.sb(f"Lm{h}", [128, 128], F32) for h in range(2)]
    Tm = [[C.sb(f"Tm{h}_{i}", [128, 128], F32) for i in range(2)] for h in range(2)]
    Ttm = [[C.sb(f"Ttm{h}_{i}", [128, 128], F32) for i in range(2)] for h in range(2)]
    Pm = [[C.sb(f"Pm{h}_{i}", [128, 128], F32) for i in range(2)] for h in range(2)]
    Ptm = [[C.sb(f"Ptm{h}_{i}", [128, 128], F32) for i in range(2)] for h in range(2)]
    Tb = [C.sb(f"Tb{h}", [128, 128], BF16) for h in range(2)]
    nwT = [C.sb(f"nwT{h}", [128, 128], BF16) for h in range(2)]
    vnb = [C.sb(f"vnb{h}", [128, 256], BF16) for h in range(2)]
    dcb = [C.sb(f"dcb{h}", [128, 2], F32) for h in range(2)]
    P = [C.ps(f"P{i}") for i in range(8)]
    QA = QAlloc(P, [[3, 4], [5, 7]])
    xTv = xT_d.rearrange("(k p) t -> p k t", p=128)
    pj_i = [0]

    def pj_slot():
        i = pj_i[0]
        pj_i[0] = (i + 1) % 2
        return P[i][:, 0:256], f'P{i}pj'

    for tt in range(NTT):
        t0 = tt * TT
        xnb = xn[tt % 2]
        xnk = f'xn{tt % 2}'
        ys = ystage[tt % 2]
        ysk = f'ystage{tt % 2}'
        for c in range(4):
            b = c % 2
            S.dma('sp', xs[b][:], xTv[:, 4 * c:4 * c + 4, t0:t0 + TT], writes=[f'xs{b}'])
            S.op('act', lambda e, b=b: e.activation(out=xsq[b][:], in_=xs[b][:], func=AF.Square),
                 reads=[f'xs{b}'], writes=[f'xsq{b}'])
            for kk in range(4):
                k = 4 * c + kk
                S.op('pool', lambda e, b=b, kk=kk, k=k: e.tensor_scalar(
                    out=xnb[:, k, :], in0=xs[b][:, kk, :], scalar1=mixw[:, k:k + 1], scalar2=0.0,
                    op0=ALU.mult, op1=ALU.add), reads=[f'xs{b}', 'mixw'], writes=[xnk])
                S.op('pe', lambda e, b=b, kk=kk, k=k: e.matmul(P[2][:, 0:TT], lhsT=ones_b[:], rhs=xsq[b][:, kk, :],
                                                               start=(k == 0), stop=(k == 15)),
                     reads=['ones_b', f'xsq{b}'], writes=['P2a'])
        S.op('act', lambda e: e.activation(out=rb[:], in_=P[2][:, 0:TT], func=AF.Ln, scale=1.0 / D_MODEL, bias=K['eps_t'][:]),
             reads=['P2a', 'eps_t'], writes=['rb'])
        S.op('act', lambda e: e.activation(out=rb[:], in_=rb[:], func=AF.Exp, scale=-0.5), reads=['rb'], writes=['rb'])
        for blk in range(NF + 1):
            ncol = 128 if blk < NF else 16
            pj, pjk = pj_slot()
            for k in range(16):
                S.op('pe', lambda e, k=k, blk=blk, ncol=ncol, pj=pj: e.matmul(
                    pj[0:ncol, :], lhsT=W[:, k, blk * 128:blk * 128 + ncol], rhs=xnb[:, k, :],
                    start=(k == 0), stop=(k == 15)), reads=[f'W{k}', xnk], writes=[pjk])
            if blk < NF:
                S.op('dve', lambda e, blk=blk, pj=pj: e.tensor_tensor(out=convin[:, blk, 3:3 + TT], in0=pj, in1=rb[:], op=ALU.mult),
                     reads=[pjk, 'rb'], writes=[f'convin{blk}'])
            else:
                S.op('dve', lambda e, pj=pj: e.tensor_tensor(out=smf[:], in0=pj[0:16, :], in1=rb[0:16, :], op=ALU.mult),
                     reads=[pjk, 'rb'], writes=['smf'])
        for blk in range(NF):
            ca = cacc[blk % 2]
            cak = f'cacc{blk % 2}'
            S.op('dve', lambda e, blk=blk, ca=ca: e.tensor_scalar(out=ca[:], in0=convin[:, blk, 0:TT], scalar1=convw[:, blk, 0:1],
                                                                 scalar2=None, op0=ALU.mult),
                 reads=[f'convin{blk}', 'convw'], writes=[cak])
            for j in range(1, 4):
                S.op('dve', lambda e, blk=blk, ca=ca, j=j: e.scalar_tensor_tensor(
                    out=ca[:], in0=convin[:, blk, j:j + TT], scalar=convw[:, blk, j:j + 1], in1=ca[:],
                    op0=ALU.mult, op1=ALU.add), reads=[f'convin{blk}', 'convw', cak], writes=[cak])
            S.op('act', lambda e, blk=blk, ca=ca: e.activation(out=so[:, blk, :], in_=ca[:], func=AF.Silu, bias=convb[:, blk:blk + 1]),
                 reads=[cak, 'convb'], writes=[f'so{blk}'])
            S.op('pool', lambda e, blk=blk: e.tensor_copy(out=convin[:, blk, 0:3], in_=convin[:, blk, TT:TT + 3]),
                 reads=[f'convin{blk}'], writes=[f'convin{blk}'])
        for sub in range(2):
            S.op('pe', lambda e, sub=sub: e.transpose(P[2][:, 256 + 16 * sub:256 + 16 * sub + 16], smf[:, sub * 128:(sub + 1) * 128], ident_f[0:16, 0:16]),
                 reads=['smf', 'ident_f'], writes=['P2b'])
        S.op('dve', lambda e: e.tensor_copy(out=smt[:], in_=P[2][:, 256:288].rearrange("p (s c) -> p s c", s=2)),
             reads=['P2b'], writes=['smt'])
        S.op('dve', lambda e: e.tensor_tensor(out=sm_tmp[:, :, 0:8], in0=smt[:, :, 0:8],
                                              in1=bc(hp[:, 0:8], 1, 2), op=ALU.add),
             reads=['smt', 'hp'], writes=['sm_tmp'])
        S.op('dve', lambda e: e.tensor_tensor(out=sm_tmp[:, :, 10:12], in0=smt[:, :, 10:12],
                                              in1=bc(hp[:, 20:22], 1, 2), op=ALU.add),
             reads=['smt', 'hp'], writes=['sm_tmp'])
        S.op('act', lambda e: e.activation(out=sm_tmp[:, :, 0:8], in_=sm_tmp[:, :, 0:8], func=AF.Exp), reads=['sm_tmp'], writes=['sm_tmp'])
        S.op('act', lambda e: e.activation(out=sm_tmp[:, :, 10:12], in_=sm_tmp[:, :, 10:12], func=AF.Exp), reads=['sm_tmp'], writes=['sm_tmp'])
        S.op('act', lambda e: e.activation(out=dt_t[:], in_=sm_tmp[:, :, 0:8], func=AF.Ln, bias=K['one_t'][:]), reads=['sm_tmp', 'one_t'], writes=['dt_t'])
        S.op('act', lambda e: e.activation(out=sm_tmp[:, :, 10:12], in_=sm_tmp[:, :, 10:12], func=AF.Ln, bias=K['one_t'][:]),
             reads=['sm_tmp', 'one_t'], writes=['sm_tmp'])
        S.op('dve', lambda e: e.tensor_tensor(out=dA_t[:], in0=dt_t[:], in1=bc(hq[:, 0:8], 1, 2), op=ALU.mult),
             reads=['dt_t', 'hq'], writes=['dA_t'])
        S.op('dve', lambda e: e.tensor_tensor(out=g_t[:], in0=sm_tmp[:, :, 10:12], in1=bc(hq[:, 8:10], 1, 2), op=ALU.mult),
             reads=['sm_tmp', 'hq'], writes=['g_t'])
        S.op('act', lambda e: e.activation(out=beta_t[:], in_=smt[:, :, 8:10], func=AF.Sigmoid), reads=['smt'], writes=['beta_t'])
        S.op('act', lambda e: e.activation(out=lnb_t[:], in_=beta_t[:], func=AF.Ln), reads=['beta_t'], writes=['lnb_t'])
        for sub in range(2):
            cs = slice(sub * 128, (sub + 1) * 128)
            for b in range(4):
                S.op('pe', lambda e, b=b: e.transpose(P[3][:, b * 128:(b + 1) * 128], so[:, b, cs], ident_f[:]),
                     reads=[f'so{b}', 'ident_f'], writes=[f'P3q{b}'])
            S.op('act', lambda e: e.copy(out=xtok[:].rearrange("p r c -> p (r c)"), in_=P[3][:]), reads=kq(3), writes=['xtok'])
            S.op('pe', lambda e: e.transpose(P[2][:, 384:512], so[:, 4, cs], ident_f[:]), reads=['so4', 'ident_f'], writes=['P2c'])
            S.op('act', lambda e: e.copy(out=btok[:], in_=P[2][:, 384:512]), reads=['P2c'], writes=['btok'])
            S.op('dve', lambda e: e.tensor_copy(out=bfb[:], in_=so[:, 4, cs]), reads=['so4'], writes=['bfb'])
            S.op('dve', lambda e: e.tensor_copy(out=cfb[:], in_=so[:, 5, cs]), reads=['so5'], writes=['cfb'])
            S.op('pe', lambda e: e.matmul(P[6][:, 0:8], lhsT=tri_f[:], rhs=dA_t[:, sub, :], start=True, stop=True),
                 reads=['tmp_ge', 'dA_t'], writes=['P6a'])
            S.op('pe', lambda e: e.matmul(P[6][:, 8:16], lhsT=ones_f[:], rhs=dA_t[:, sub, :], start=True, stop=True),
                 reads=['ones_f', 'dA_t'], writes=['P6b'])
            S.op('dve', lambda e: e.tensor_copy(out=acs[:], in_=P[6][:, 0:8]), reads=['P6a'], writes=['acs'])
            S.op('dve', lambda e: e.tensor_scalar(out=nacs[:], in0=P[6][:, 0:8], scalar1=-1.0, scalar2=None, op0=ALU.mult),
                 reads=['P6a'], writes=['nacs'])
            S.op('dve', lambda e: e.tensor_tensor(out=tend[:], in0=P[6][:, 8:16], in1=acs[:], op=ALU.subtract),
                 reads=['P6b', 'acs'], writes=['tend'])
            S.op('act', lambda e: e.activation(out=tend[:], in_=tend[:], func=AF.Exp), reads=['tend'], writes=['tend'])
            S.op('act', lambda e: e.activation(out=cdec[:], in_=P[6][:, 8:16], func=AF.Exp), reads=['P6b'], writes=['cdec'])
            S.op('dve', lambda e: e.tensor_tensor(out=wsc[:], in0=dt_t[:, sub, :], in1=tend[:], op=ALU.mult),
                 reads=['dt_t', 'tend'], writes=['wsc'])
            S.op('dve', lambda e: e.tensor_tensor(out=xdt[:], in0=xtok[:], in1=bc(dt_t[:, sub, :], 2, 64), op=ALU.mult),
                 reads=['xtok', 'dt_t'], writes=['xdt'])
            S.op('dve', lambda e: e.tensor_tensor(out=xdtw[:], in0=xtok[:], in1=bc(wsc[:], 2, 64), op=ALU.mult),
                 reads=['xtok', 'wsc'], writes=['xdtw'])
            S.op('pool', lambda e: e.tensor_tensor(out=Xda[:], in0=bc(tri_f[:], 1, 8),
                                                   in1=bc(dA_t[:, sub, :], 2, 128), op=ALU.mult),
                 reads=['tmp_ge', 'dA_t'], writes=['Xda'])
            for hf in range(2):
                S.op('pe', lambda e, hf=hf: e.matmul(P[4 + hf][:], lhsT=ones_f[:], rhs=Xda[:, 4 * hf:4 * hf + 4, :].rearrange("p r l -> p (r l)"),
                                                     start=True, stop=False), reads=['ones_f', 'Xda'], writes=kq(4 + hf))
                S.op('pe', lambda e, hf=hf: e.matmul(P[4 + hf][:], lhsT=ident_f[:], rhs=nmrep[:, 4 * hf:4 * hf + 4, :].rearrange("p r l -> p (r l)"),
                                                     start=False, stop=True), reads=['ident_f', 'nmrep'], writes=kq(4 + hf))
            for r in range(8):
                S.op('act', lambda e, r=r: e.activation(out=decay[:, r, :], in_=P[4 + r // 4][:, (r % 4) * 128:(r % 4 + 1) * 128],
                                                        func=AF.Exp, bias=nacs[:, r:r + 1]),
                     reads=[f'P{4 + r // 4}q{r % 4}', 'nacs'], writes=['decay'])
            S.op('pe', lambda e: e.matmul(P[6][:, 128:256], lhsT=bfb[:], rhs=cfb[:], start=True, stop=True), reads=['bfb', 'cfb'], writes=['P6c'])
            S.op('dve', lambda e: e.tensor_tensor(out=scor[:], in0=decay[:], in1=bc(P[6][:, 128:256], 1, 8), op=ALU.mult),
                 reads=['decay', 'P6c'], writes=['scor'])
            for hf in range(2):
                S.op('pe', lambda e, hf=hf: e.matmul(P[4 + hf][:], lhsT=ones_f[:], rhs=Xda[:, 4 * hf:4 * hf + 4, :].rearrange("p r l -> p (r l)"),
                                                     start=True, stop=True), reads=['ones_f', 'Xda'], writes=kq(4 + hf))
                S.op('act', lambda e, hf=hf: e.activation(out=Eb[:, 4 * hf:4 * hf + 4, :].rearrange("p r l -> p (r l)"), in_=P[4 + hf][:], func=AF.Exp),
                     reads=kq(4 + hf), writes=['Eb'])
            S.op('pool', lambda e: e.tensor_tensor(out=Cdec[:], in0=Eb[:], in1=bc(so[:, 5, cs], 1, 8), op=ALU.mult),
                 reads=['Eb', 'so5'], writes=['Cdec'])
            for r in range(8):
                b, half = r // 2, r % 2
                out = P[7][half * 64:(half + 1) * 64, b * 128:(b + 1) * 128]
                S.op('pe', lambda e, r=r, out=out: e.matmul(out, lhsT=xdt[:, r, :], rhs=scor[:, r, :], start=True, stop=False),
                     reads=['xdt', 'scor'], writes=[f'P7q{b}'])
                S.op('pe', lambda e, r=r, out=out: e.matmul(out, lhsT=h_b[:, r * 64:(r + 1) * 64], rhs=Cdec[:, r, :], start=False, stop=True),
                     reads=['h_b', 'Cdec'], writes=[f'P7q{b}'])
            for b in range(4):
                S.op('dve', lambda e, b=b: e.scalar_tensor_tensor(out=ys[:, b, cs], in0=so[:, b, cs], scalar=hp[:, 16 + b:17 + b],
                                                                 in1=P[7][:, b * 128:(b + 1) * 128], op0=ALU.mult, op1=ALU.add),
                     reads=[f'so{b}', 'hp', f'P7q{b}'], writes=[ysk])
            S.op('pe', lambda e: e.matmul(P[6][:, :], lhsT=btok[:], rhs=xdtw[:].rearrange("p r c -> p (r c)"), start=True, stop=True),
                 reads=['btok', 'xdtw'], writes=['P6a', 'P6b', 'P6c'])
            S.op('dve', lambda e: e.tensor_tensor(out=h_f[:], in0=h_f[:], in1=bc(cdec[:], 2, 64), op=ALU.mult),
                 reads=['h_f', 'cdec'], writes=['h_f'])
            S.op('dve', lambda e: e.tensor_tensor(out=h_f[:], in0=h_f[:], in1=P[6][:].rearrange("p (r c) -> p r c", r=8), op=ALU.add),
                 reads=['h_f', 'P6a', 'P6b', 'P6c'], writes=['h_f'])
            S.op('act', lambda e: e.copy(out=h_b[:], in_=h_f[:].rearrange("p r c -> p (r c)")), reads=['h_f'], writes=['h_b'])
        for sub in range(2):
            gdn_pair(C, K, P, QA, so, ys, ysk, sub, g_t, beta_t, lnb_t, Sg_f, Sg_b, gsq, rn, kn, qn, qd, kb, kdec, vb, gsc,
                     Xg, Xg2, Eg, Ee, Es, attnT, Mm, Lm, Tm, Ttm, Pm, Ptm, Tb, nwT, vnb, dcb)
        store(tt, ys, ysk)


def build_p1(T):
    nc = bass.Bass("TRN2", target_bir_lowering=False)
    D = p1_decl(nc, T)
    y_d = nc.dram_tensor("y1T", [1024, T], BF16, kind="ExternalOutput").ap()
    yv = y_d.rearrange("(b p) t -> p b t", p=128)
    with ExitStack() as es:
        C = Ctx(nc, es)
        K = build_consts(C)
        p1_body(C, K, T, D, lambda tt, ys, ysk: C.S.dma('sp', yv[:, :, tt * TT:(tt + 1) * TT], ys[:], reads=[ysk], writes=['y_out']))
        C.S.barrier()
    return nc


def prep_p1(inp, g, T):
    w_in = inp["w_in"][0]
    base = SSM_PROJ
    cols = np.concatenate([
        4096 + g * 512 + np.arange(512),
        4096 + 4096 + g * 128 + np.arange(128),
        4096 + 4096 + 1024 + g * 128 + np.arange(128),
        base + 2 * g * 128 + np.arange(256),
        base + 2048 + 2 * g * 128 + np.arange(256),
        base + 4096 + 2 * g * 256 + np.arange(512),
        10240 + g * 8 + np.arange(8),
        base + 12288 + 2 * g + np.arange(2),
        base + 12288 + 16 + 2 * g + np.arange(2),
    ])
    w1 = np.zeros((D_MODEL, NCOL1), np.float32)
    w1[:, :cols.size] = w_in[:, cols]
    scw, scb, gcw = inp["ssm_conv_w"][0], inp["ssm_conv_b"][0], inp["gdn_conv_w"][0]
    cw = np.concatenate([
        scw[:, g * 512:(g + 1) * 512], scw[:, 4096 + g * 128:4096 + (g + 1) * 128],
        scw[:, 5120 + g * 128:5120 + (g + 1) * 128],
        gcw[:, 2 * g * 128:2 * g * 128 + 256], gcw[:, 2048 + 2 * g * 128:2048 + 2 * g * 128 + 256],
        gcw[:, 4096 + 2 * g * 256:4096 + 2 * g * 256 + 512]], axis=1)
    convw = np.ascontiguousarray(cw.reshape(4, NF, 128).transpose(2, 1, 0))
    cb = np.zeros(NF * 128, np.float32)
    cb[0:512] = scb[g * 512:(g + 1) * 512]
    cb[512:640] = scb[4096 + g * 128:4096 + (g + 1) * 128]
    cb[640:768] = scb[5120 + g * 128:5120 + (g + 1) * 128]
    convb = np.ascontiguousarray(cb.reshape(NF, 128).T)
    hp = np.zeros((128, 32), np.float32)
    hp[:, 0:8] = inp["ssm_dt_bias"][0][g * 8:(g + 1) * 8]
    hp[:, 8:16] = inp["ssm_a_log"][0][g * 8:(g + 1) * 8]
    dd = inp["ssm_d"][0][g * 8:(g + 1) * 8]
    for b in range(4):
        hp[0:64, 16 + b] = dd[2 * b]
        hp[64:128, 16 + b] = dd[2 * b + 1]
    hp[:, 20:22] = inp["gdn_dt_bias"][0][2 * g:2 * g + 2]
    hp[:, 22:24] = inp["gdn_a_log"][0][2 * g:2 * g + 2]
    mixw = np.ascontiguousarray(inp["mix_norm_w"][0].reshape(16, 128).T)
    return {"w1": w1, "convw": convw, "convb": convb, "hp": hp, "mixw": mixw}


def peer_group(C, K, P, PA, h1, nwb_d, wq_d, skT, u4_d, v_d, neb):
    S = C.S
    ones_b, ident_b = K['ones_b'], K['ident_b']
    C.push()
    hnT = C.sb("hnT", [128, 16, TG], BF16)
    s_sb = C.sb("s_sb", [128, 4, 16, 128], F32)
    kap = C.sb("kap", [128, 4, 8], F32)
    C.push()
    ffw = C.sb("ffw", [128, D_MODEL], F32)
    hn = C.sb("hn", [128, 4, D_MODEL], BF16)
    qT = C.sb("qT", [128, 16, TG], BF16)
    wqc = [C.sb(f"wqc{i}", [128, 16, 512], BF16) for i in range(2)]
    ss4 = C.sb("ss4", [128, 4], F32)
    junk = C.sb("junk2", [128, D_MODEL], BF16)
    tops = C.sb("tops", [128, 2, 16], F32)
    sc = C.sb("sc", [128, 128], F32)
    cand = C.sb("cand", [128, 256], F32)
    cand2 = C.sb("cand2", [128, 256], F32)
    ctop = C.sb("ctop", [128, 16], F32)
    ex16 = C.sb("ex16", [128, 16], F32)
    sm = C.sb("sm", [128, 8], F32)
    S.dma('sp', ffw[:], nwb_d[:, 0, :], writes=['ffw'])
    for ts in range(4):
        S.op('act', lambda e: e.activation(out=junk[:], in_=h1[:, ts, :], func=AF.Square, accum_out=ss4[:, ts:ts + 1]),
             reads=['h1'], writes=['junk2', 'ss4'])
    S.op('act', lambda e: e.activation(out=ss4[:], in_=ss4[:], func=AF.Ln, scale=1.0 / D_MODEL, bias=K['eps_t'][:]), reads=['ss4', 'eps_t'], writes=['ss4'])
    S.op('act', lambda e: e.activation(out=ss4[:], in_=ss4[:], func=AF.Exp, scale=-0.5), reads=['ss4'], writes=['ss4'])
    for ts in range(4):
        S.op('dve', lambda e: e.scalar_tensor_tensor(out=hn[:, ts, :], in0=h1[:, ts, :], scalar=ss4[:, ts:ts + 1], in1=ffw[:],
                                                    op0=ALU.mult, op1=ALU.mult), reads=['h1', 'ss4', 'ffw'], writes=[f'hn{ts}'])
    n = 0
    for ts in range(4):
        for kg in range(4):
            b = [0, 3][n % 2]
            n += 1
            for kk in range(4):
                k = 4 * kg + kk
                S.op('pe', lambda e: e.matmul(P[b][:, kk * 128:(kk + 1) * 128], lhsT=hn[:, ts, k * 128:(k + 1) * 128], rhs=ident_b[:], start=True, stop=True),
                     reads=[f'hn{ts}', 'ident_b'], writes=[f'P{b}t'])
            if n % 2:
                S.op('act', lambda e: e.copy(out=hnT[:, 4 * kg:4 * kg + 4, ts * 128:(ts + 1) * 128], in_=P[b][:].rearrange("p (a t) -> p a t", a=4)),
                     reads=[f'P{b}t'], writes=['hnT'])
            else:
                S.op('dve', lambda e: e.tensor_copy(out=hnT[:, 4 * kg:4 * kg + 4, ts * 128:(ts + 1) * 128], in_=P[b][:].rearrange("p (a t) -> p a t", a=4)),
                     reads=[f'P{b}t'], writes=['hnT'])
    for cg in range(4):
        S.dma('pool', wqc[cg % 2][:], wq_d[cg], writes=[f'wqc{cg % 2}'])
        for cb in range(4):
            b = 1 + (cb % 2)
            for k in range(16):
                S.op('pe', lambda e: e.matmul(P[b][:], lhsT=wqc[cg % 2][:, k, cb * 128:(cb + 1) * 128], rhs=hnT[:, k, :], start=(k == 0), stop=(k == 15)),
                     reads=[f'wqc{cg % 2}', 'hnT'], writes=[f'P{b}z'])
            S.op('act', lambda e: e.copy(out=qT[:, 4 * cg + cb, :], in_=P[b][:]), reads=[f'P{b}z'], writes=[f'qT{4 * cg + cb}'])
    n = 0
    for ts in range(4):
        for c4 in range(4):
            b = [0, 3][n % 2]
            n += 1
            for j in range(4):
                c = 4 * c4 + j
                S.op('pe', lambda e: e.matmul(P[b][:, j * 128:(j + 1) * 128], lhsT=qT[:, c, ts * 128:(ts + 1) * 128], rhs=skT[:, c, :], start=True, stop=True),
                     reads=[f'qT{c}', 'skT'], writes=[f'P{b}t'])
            S.op('dve', lambda e: e.tensor_copy(out=s_sb[:, ts, 4 * c4:4 * c4 + 4, :], in_=P[b][:].rearrange("p (a t) -> p a t", a=4)),
                 reads=[f'P{b}t'], writes=[f's_sb{ts}'])
    for ts in range(4):
        sk = f's_sb{ts}'
        for h in range(8):
            for pp in range(2):
                src = s_sb[:, ts, 2 * h + pp, :]
                S.op('dve', lambda e: e.max(out=tops[:, pp, 0:8], in_=src), reads=[sk], writes=['tops'])
                S.op('dve', lambda e: e.match_replace(out=sc[:], in_to_replace=tops[:, pp, 0:8], in_values=src, imm_value=-1e30),
                     reads=[sk, 'tops'], writes=['sc'])
                S.op('dve', lambda e: e.max(out=tops[:, pp, 8:16], in_=sc[:]), reads=['sc'], writes=['tops'])
            S.op('dve', lambda e: e.tensor_tensor(out=cand[:].rearrange("p (a b) -> p a b", a=16), in0=bc(tops[:, 0, :], 2, 16), in1=bc(tops[:, 1, :], 1, 16), op=ALU.add),
                 reads=['tops'], writes=['cand'])
            S.op('dve', lambda e: e.max(out=ctop[:, 0:8], in_=cand[:]), reads=['cand'], writes=['ctop'])
            S.op('dve', lambda e: e.match_replace(out=cand2[:], in_to_replace=ctop[:, 0:8], in_values=cand[:], imm_value=-1e30),
                 reads=['cand', 'ctop'], writes=['cand2'])
            S.op('dve', lambda e: e.max(out=ctop[:, 8:16], in_=cand2[:]), reads=['cand2'], writes=['ctop'])
            S.op('dve', lambda e: e.tensor_scalar(out=sm[:, 0:1], in0=ctop[:, 0:1], scalar1=-1.0, scalar2=None, op0=ALU.mult), reads=['ctop'], writes=['sm'])
            S.op('act', lambda e: e.activation(out=ex16[:], in_=ctop[:], func=AF.Exp, bias=sm[:, 0:1], accum_out=sm[:, 1:2]),
                 reads=['ctop', 'sm'], writes=['ex16', 'sm'])
            S.op('act', lambda e: e.activation(out=sm[:, 2:3], in_=sm[:, 1:2], func=AF.Ln), reads=['sm'], writes=['sm'])
            S.op('dve', lambda e: e.tensor_tensor(out=sm[:, 3:4], in0=sm[:, 0:1], in1=sm[:, 2:3], op=ALU.subtract), reads=['sm'], writes=['sm'])
            S.op('dve', lambda e: e.tensor_scalar(out=s_sb[:, ts, 2 * h, :], in0=s_sb[:, ts, 2 * h, :], scalar1=sm[:, 3:4], scalar2=None, op0=ALU.add),
                 reads=[sk, 'sm'], writes=[sk])
            S.op('dve', lambda e: e.tensor_scalar(out=sm[:, 4:5], in0=ctop[:, 15:16], scalar1=sm[:, 3:4], scalar2=-1e-4, op0=ALU.add, op1=ALU.add),
                 reads=['ctop', 'sm'], writes=['sm'])
            S.op('act', lambda e: e.activation(out=kap[:, ts, h:h + 1], in_=sm[:, 4:5], func=AF.Exp), reads=['sm'], writes=['kap'])
    C.pop()
    C.push()
    uT = [C.sb(f"uT{i}", [128, 16, 128], BF16) for i in range(3)]
    vt = [C.sb(f"vt{i}", [128, D_MODEL], BF16) for i in range(2 * GRP)]
    gS = [C.sb(f"gS{i}", [128, TG], F32) for i in range(2)]
    Eb = [C.sb(f"Eb{i}", [128, 8, 128], F32) for i in range(2)]
    Gb = [C.sb(f"Gb{i}", [128, 8, 128], BF16) for i in range(2)]
    AT = [C.sb(f"AT{i}", [128, GRP, TG], BF16) for i in range(2)]
    e2 = 0
    for grp in range(neb // GRP):
        a2 = grp % 2
        for gi_ in range(GRP):
            i = grp * GRP + gi_
            u_, uk = uT[i % 3], f'uT{i % 3}'
            v_, vk = vt[i % (2 * GRP)], f'vt{i % (2 * GRP)}'
            S.dma('pool', u_[:], u4_d[i], writes=[uk])
            S.dma('pool', v_[:], v_d[i * 128:(i + 1) * 128, :], writes=[vk])
            sb_ = 1 + (i % 2)
            for k in range(16):
                S.op('pe', lambda e: e.matmul(P[sb_][:], lhsT=u_[:, k, :], rhs=hnT[:, k, :], start=(k == 0), stop=(k == 15)),
                     reads=[uk, 'hnT'], writes=[f'P{sb_}z'])
            S.op('act', lambda e: e.activation(out=gS[i % 2][:], in_=P[sb_][:], func=AF.Gelu), reads=[f'P{sb_}z'], writes=[f'gS{i % 2}'])
            gb_ = [0, 3][i % 2]
            for ts in range(4):
                for h in range(8):
                    S.op('act', lambda e: e.activation(out=Eb[e2][:, h, :], in_=s_sb[:, ts, 2 * h + 1, :], func=AF.Exp, bias=s_sb[:, ts, 2 * h, i:i + 1]),
                         reads=[f's_sb{ts}'], writes=[f'Eb{e2}'])
                for h in range(8):
                    S.op('dve', lambda e: e.scalar_tensor_tensor(out=Gb[e2][:, h, :], in0=Eb[e2][:, h, :], scalar=kap[:, ts, h:h + 1], in1=Eb[e2][:, h, :],
                                                                op0=ALU.is_ge, op1=ALU.mult), reads=[f'Eb{e2}', 'kap'], writes=[f'Gb{e2}'])
                for h in range(8):
                    S.op('pe', lambda e: e.matmul(P[gb_][:, ts * 128:(ts + 1) * 128], lhsT=Gb[e2][:, h, :], rhs=ident_b[:], start=(h == 0), stop=(h == 7)),
                         reads=[f'Gb{e2}', 'ident_b'], writes=[f'P{gb_}t'])
                e2 = 1 - e2
            S.op('dve', lambda e: e.tensor_tensor(out=AT[a2][:, gi_, :], in0=P[gb_][:], in1=gS[i % 2][:], op=ALU.mult),
                 reads=[f'P{gb_}t', f'gS{i % 2}'], writes=[f'AT{a2}'])
        for ts in range(4):
            for dc in range(4):
                for gi_ in range(GRP):
                    i = grp * GRP + gi_
                    S.op('pe', lambda e: e.matmul(PA[:, dc * 512:(dc + 1) * 512], lhsT=AT[a2][:, gi_, ts * 128:(ts + 1) * 128],
                                                  rhs=vt[i % (2 * GRP)][:, dc * 512:(dc + 1) * 512], start=(gi_ == 0), stop=(gi_ == GRP - 1)),
                         reads=[f'AT{a2}', f'vt{i % (2 * GRP)}'], writes=[f'P{4 + dc}acc'])
            S.op('dve', lambda e: e.tensor_tensor(out=h1[:, ts, :], in0=PA[:], in1=h1[:, ts, :], op=ALU.add),
                 reads=[f'P{4 + d}acc' for d in range(4)] + ['h1'], writes=['h1'])
    C.pop()
    C.pop()

TG = 512
GRP = 4
NEB = 128


def tile_w(Wm, chunk):
    Kd, N = Wm.shape
    return np.ascontiguousarray(Wm.reshape(Kd // 128, 128, N // chunk, chunk).transpose(2, 1, 0, 3))


def p2_decl(nc, TL, xtn="xT"):
    return dict(
        xT=nc.dram_tensor(xtn, [D_MODEL, TL], F32, kind="ExternalInput").ap(),
        xtok=nc.dram_tensor("xtok", [TL, D_MODEL], F32, kind="ExternalInput").ap(),
        wzg=nc.dram_tensor("wzg", [32, 128, 16, 256], F32, kind="ExternalInput").ap(),
        wgt=nc.dram_tensor("wgt", [32, 128, 16, 128], F32, kind="ExternalInput").ap(),
        wbs=nc.dram_tensor("wbs", [16, 128, 32, 128], F32, kind="ExternalInput").ap(),
        wbg=nc.dram_tensor("wbg", [16, 128, 32, 128], F32, kind="ExternalInput").ap(),
        wout=nc.dram_tensor("wout", [8, 128, 16, 256], F32, kind="ExternalInput").ap(),
        wq=nc.dram_tensor("wq", [4, 128, 16, 512], F32, kind="ExternalInput").ap(),
        skT=nc.dram_tensor("skT", [128, 16, 128], F32, kind="ExternalInput").ap(),
        u4=nc.dram_tensor("u4", [NEB, 128, 16, 128], F32, kind="ExternalInput").ap(),
        v=nc.dram_tensor("vtab", [NEB * 128, D_MODEL], F32, kind="ExternalInput").ap(),
        sp=nc.dram_tensor("sp2", [128, 128], F32, kind="ExternalInput").ap(),
        nwb=nc.dram_tensor("nwb", [128, 2, D_MODEL], F32, kind="ExternalInput").ap(),
        out=nc.dram_tensor("out", [TL, D_MODEL], F32, kind="ExternalOutput").ap(),)


def p2_body(C, K, TL, D, yload, neb=NEB):
    S = C.S
    NG = TL // TG
    xT_d = D['xT']
    xtok_d = D['xtok']
    wzg_d = D['wzg']
    wgt_d = D['wgt']
    wbs_d = D['wbs']
    wbg_d = D['wbg']
    wout_d = D['wout']
    wq_d = D['wq']
    skT_d = D['skT']
    u4_d = D['u4']
    v_d = D['v']
    sp_d = D['sp']
    nwb_d = D['nwb']
    out_d = D['out']
    ones_b, ident_b = K['ones_b'], K['ident_b']
    P = [C.ps(f"P{i}") for i in range(4)]
    PA = C.ps("PA", [128, 2048], F32)
    spm = C.sb("spm", [128, 128], F32)
    S.dma('sp', spm[:], sp_d, writes=['spm'])
    skT = C.sb("skT_s", [128, 16, 128], BF16)
    S.dma('pool', skT[:], skT_d, writes=['skT'])
    h1 = C.sb("h1", [128, 4, D_MODEL], F32)
    xTv = xT_d.rearrange("(k p) t -> p k t", p=128)
    xtv = xtok_d.rearrange("(s p) d -> p s d", p=128)
    outv = out_d.rearrange("(s p) d -> p s d", p=128)
    pi = [0]

    def pbank():
        i = pi[0]
        pi[0] = (i + 1) % 2
        return P[1 + i], f'P{1 + i}z'

    def rsq(out, in_, scale, reads, writes):
        S.op('act', lambda e: e.activation(out=out, in_=in_, func=AF.Ln, scale=scale, bias=K['eps_t'][:]), reads=reads + ['eps_t'], writes=writes)
        S.op('act', lambda e: e.activation(out=out, in_=out, func=AF.Exp, scale=-0.5), reads=writes, writes=writes)

    for gi in range(NG):
        tg0 = gi * TG
        tsl = slice(tg0, tg0 + TG)
        S.dma('sp', h1[:], xtv[:, 4 * gi:4 * gi + 4, :], writes=['h1'])
        C.push()
        xn = C.sb("xn", [128, 16, TG], BF16)
        rb = C.sb("rb", [128, TG], F32)
        Yn = C.sb("Yn", [128, 64, TG], BF16)
        C.push()
        xs = [C.sb(f"xs{i}", [128, 4, TG], F32) for i in range(2)]
        xsq = [C.sb(f"xsq{i}", [128, 4, TG], BF16) for i in range(2)]
        Wzc = [C.sb(f"Wzc{i}", [128, 16, 256], BF16) for i in range(2)]
        Yr = [C.sb(f"Yr{i}", [128, 4, TG], BF16) for i in range(2)]
        zt = [C.sb(f"zt{i}", [128, TG], F32) for i in range(2)]
        yg = C.sb("yg", [128, 4, TG], F32)
        sqb = [C.sb(f"sqb{i}", [128, TG], BF16) for i in range(2)]
        rgb = C.sb("rgb", [128, TG], F32)
        for c in range(4):
            b = c % 2
            S.dma('sp', xs[b][:], xTv[:, 4 * c:4 * c + 4, tsl], writes=[f'xs{b}'])
            S.op('act', lambda e: e.activation(out=xsq[b][:], in_=xs[b][:], func=AF.Square), reads=[f'xs{b}'], writes=[f'xsq{b}'])
            for kk in range(4):
                k = 4 * c + kk
                S.op('pool', lambda e: e.tensor_scalar(out=xn[:, k, :], in0=xs[b][:, kk, :], scalar1=spm[:, k:k + 1], scalar2=0.0,
                                                       op0=ALU.mult, op1=ALU.add), reads=[f'xs{b}', 'spm'], writes=['xn'])
                S.op('pe', lambda e: e.matmul(P[0][:], lhsT=ones_b[:], rhs=xsq[b][:, kk, :], start=(k == 0), stop=(k == 15)),
                     reads=['ones_b', f'xsq{b}'], writes=['P0ss'])
        rsq(rb[:], P[0][:], 1.0 / D_MODEL, ['P0ss'], ['rb'])
        wi = [0]

        def load_wz(chunk):
            i = wi[0]
            wi[0] = (i + 1) % 2
            S.dma('pool', Wzc[i][:], wzg_d[chunk], writes=[f'Wzc{i}'])
            return Wzc[i], f'Wzc{i}'

        def zproj(Wt, Wk, cb):
            pb, pk = pbank()
            for k in range(16):
                S.op('pe', lambda e: e.matmul(pb[:], lhsT=Wt[:, k, cb * 128:(cb + 1) * 128], rhs=xn[:, k, :], start=(k == 0), stop=(k == 15)),
                     reads=[Wk, 'xn'], writes=[pk])
            j = zproj.i
            zproj.i = (j + 1) % 2
            S.op('dve', lambda e: e.tensor_tensor(out=zt[j][:], in0=pb[:], in1=rb[:], op=ALU.mult), reads=[pk, 'rb'], writes=[f'zt{j}'])
            S.op('act', lambda e: e.activation(out=zt[j][:], in_=zt[j][:], func=AF.Silu), reads=[f'zt{j}'], writes=[f'zt{j}'])
            return zt[j], f'zt{j}'
        zproj.i = 0
        for gg in range(8):
            yb = gg % 2
            S.dma('sp', Yr[yb][:], yload(4 * gg, 4 * gg + 4, tsl), reads=['yown'], writes=[f'Yr{yb}'])
            for b in range(4):
                if b % 2 == 0:
                    Wt, Wk = load_wz(2 * gg + b // 2)
                z_, zk = zproj(Wt, Wk, b % 2)
                S.op('dve', lambda e: e.tensor_tensor(out=yg[:, b, :], in0=Yr[yb][:, b, :], in1=z_[:], op=ALU.mult),
                     reads=[f'Yr{yb}', zk], writes=[f'yg{b}'])
                S.op('act', lambda e: e.activation(out=sqb[b % 2][:], in_=yg[:, b, :], func=AF.Square), reads=[f'yg{b}'], writes=[f'sqb{b % 2}'])
                S.op('pe', lambda e: e.matmul(P[3][:], lhsT=ones_b[:], rhs=sqb[b % 2][:], start=(b == 0), stop=(b == 3)),
                     reads=['ones_b', f'sqb{b % 2}'], writes=['P3ss'])
            rsq(rgb[:], P[3][:], 1.0 / 512, ['P3ss'], ['rgb'])
            for b in range(4):
                blk = 4 * gg + b
                S.op('dve', lambda e: e.scalar_tensor_tensor(out=Yn[:, blk, :], in0=yg[:, b, :], scalar=spm[:, 48 + blk:49 + blk], in1=rgb[:],
                                                            op0=ALU.mult, op1=ALU.mult), reads=[f'yg{b}', 'spm', 'rgb'], writes=[f'Yn{blk}'])
        for hh in range(16):
            yb = (hh // 2) % 2
            o2 = 2 * (hh % 2)
            if hh % 2 == 0:
                S.dma('sp', Yr[yb][:], yload(32 + 2 * hh, 32 + 2 * hh + 4, tsl), reads=['yown'], writes=[f'Yr{yb}'])
            Wt, Wk = load_wz(16 + hh)
            for b in range(2):
                S.op('act', lambda e: e.activation(out=sqb[b][:], in_=Yr[yb][:, o2 + b, :], func=AF.Square), reads=[f'Yr{yb}'], writes=[f'sqb{b}'])
                S.op('pe', lambda e: e.matmul(P[3][:], lhsT=ones_b[:], rhs=sqb[b][:], start=(b == 0), stop=(b == 1)),
                     reads=['ones_b', f'sqb{b}'], writes=['P3ss'])
            rsq(rgb[:], P[3][:], 1.0 / 256, ['P3ss'], ['rgb'])
            for b in range(2):
                blk = 32 + 2 * hh + b
                z_, zk = zproj(Wt, Wk, b)
                S.op('dve', lambda e: e.scalar_tensor_tensor(out=yg[:, b, :], in0=Yr[yb][:, o2 + b, :], scalar=spm[:, 80 + b:81 + b], in1=rgb[:],
                                                            op0=ALU.mult, op1=ALU.mult), reads=[f'Yr{yb}', 'spm', 'rgb'], writes=[f'yg{b}'])
                S.op('pool', lambda e: e.tensor_tensor(out=Yn[:, blk, :], in0=yg[:, b, :], in1=z_[:], op=ALU.mult),
                     reads=[f'yg{b}', zk], writes=[f'Yn{blk}'])
        C.pop()
        C.push()
        Wg = [C.sb(f"Wg{i}", [128, 16, 128], BF16) for i in range(2)]
        Wbs = [C.sb(f"Wbs{i}", [128, 32, 128], BF16) for i in range(2)]
        Wbg = [C.sb(f"Wbg{i}", [128, 32, 128], BF16) for i in range(2)]
        gte = [C.sb(f"gte{i}", [128, TG], F32) for i in range(2)]
        tm = [C.sb(f"tm{i}", [128, TG], F32) for i in range(2)]
        mixb = C.sb("mixb", [128, 16, TG], BF16)
        woc = [C.sb(f"woc{i}", [128, 16, 256], BF16) for i in range(2)]
        for mb in range(16):
            for br in range(2):
                S.dma('pool', Wg[br][:], wgt_d[br * 16 + mb], writes=[f'Wg{br}'])
            i2 = mb % 2
            S.dma('pool', Wbs[i2][:], wbs_d[mb], writes=[f'Wbs{i2}'])
            S.dma('pool', Wbg[i2][:], wbg_d[mb], writes=[f'Wbg{i2}'])
            for br in range(2):
                pb, pk = pbank()
                for k in range(16):
                    S.op('pe', lambda e: e.matmul(pb[:], lhsT=Wg[br][:, k, :], rhs=xn[:, k, :],
                                                  start=(k == 0), stop=(k == 15)), reads=[f'Wg{br}', 'xn'], writes=[pk])
                S.op('dve', lambda e: e.tensor_tensor(out=gte[br][:], in0=pb[:], in1=rb[:], op=ALU.mult), reads=[pk, 'rb'], writes=[f'gte{br}'])
                S.op('act', lambda e: e.activation(out=gte[br][:], in_=gte[br][:], func=AF.Sigmoid, bias=spm[:, 16 + br * 16 + mb:17 + br * 16 + mb]),
                     reads=[f'gte{br}', 'spm'], writes=[f'gte{br}'])
            for br, Wb_ in ((0, Wbs), (1, Wbg)):
                pb, pk = pbank()
                for cb in range(32):
                    S.op('pe', lambda e: e.matmul(pb[:], lhsT=Wb_[i2][:, cb, :], rhs=Yn[:, 32 * br + cb, :], start=(cb == 0), stop=(cb == 31)),
                         reads=[f'Wb{"sg"[br]}{i2}', f'Yn{32 * br + cb}'], writes=[pk])
                S.op('dve', lambda e: e.tensor_tensor(out=tm[br][:], in0=pb[:], in1=gte[br][:], op=ALU.mult), reads=[pk, f'gte{br}'], writes=[f'tm{br}'])
            S.op('pool', lambda e: e.tensor_tensor(out=mixb[:, mb, :], in0=tm[0][:], in1=tm[1][:], op=ALU.add), reads=['tm0', 'tm1'], writes=['mixb'])
        for dc in range(8):
            S.dma('pool', woc[dc % 2][:], wout_d[dc], writes=[f'woc{dc % 2}'])
            for ts in range(4):
                pb, pk = pbank()
                for mb in range(16):
                    S.op('pe', lambda e: e.matmul(pb[:, 0:256], lhsT=mixb[:, mb, ts * 128:(ts + 1) * 128], rhs=woc[dc % 2][:, mb, :],
                                                  start=(mb == 0), stop=(mb == 15)), reads=['mixb', f'woc{dc % 2}'], writes=[pk])
                S.op('dve', lambda e: e.tensor_tensor(out=h1[:, ts, dc * 256:(dc + 1) * 256], in0=pb[:, 0:256], in1=h1[:, ts, dc * 256:(dc + 1) * 256], op=ALU.add),
                     reads=[pk, 'h1'], writes=['h1'])
        C.pop()
        C.pop()
        peer_group(C, K, P, PA, h1, nwb_d, wq_d, skT, u4_d, v_d, neb)
        C.push()
        fnw = C.sb("fnw", [128, D_MODEL], F32)
        ssf = C.sb("ssf", [128, 4], F32)
        junk = C.sb("junk", [128, D_MODEL], BF16)
        S.dma('sp', fnw[:], nwb_d[:, 1, :], writes=['fnw'])
        for ts in range(4):
            S.op('act', lambda e: e.activation(out=junk[:], in_=h1[:, ts, :], func=AF.Square, accum_out=ssf[:, ts:ts + 1]),
                 reads=['h1'], writes=['junk', 'ssf'])
        rsq(ssf[:], ssf[:], 1.0 / D_MODEL, ['ssf'], ['ssf'])
        for ts in range(4):
            S.op('dve', lambda e: e.scalar_tensor_tensor(out=h1[:, ts, :], in0=h1[:, ts, :], scalar=ssf[:, ts:ts + 1], in1=fnw[:],
                                                        op0=ALU.mult, op1=ALU.mult), reads=['h1', 'ssf', 'fnw'], writes=['h1'])
        S.dma('sp', outv[:, 4 * gi:4 * gi + 4, :], h1[:], reads=['h1'], writes=['out'])
        C.pop()


def build_p2(TL, neb=NEB):
    nc = bass.Bass("TRN2", target_bir_lowering=False)
    YT_d = nc.dram_tensor("YT", [8192, TL], BF16, kind="ExternalInput").ap()
    YTv = YT_d.rearrange("(b p) t -> p b t", p=128)
    D = p2_decl(nc, TL)
    with ExitStack() as es:
        C = Ctx(nc, es)
        K = build_consts(C)
        p2_body(C, K, TL, D, lambda a, b, tsl: YTv[:, a:b, tsl], neb)
        C.S.barrier()
    return nc


def prep_p2_shared(inp):
    w_in = inp["w_in"][0]
    base = SSM_PROJ
    wz = np.concatenate([w_in[:, 0:4096], w_in[:, base + 8192:base + 8192 + 4096]], axis=1)
    sp2 = np.zeros((128, 128), np.float32)
    sp2[:, 0:16] = inp["mix_norm_w"][0].reshape(16, 128).T
    sp2[:, 16:48] = inp["gate_b"][0].reshape(2, 16, 128).transpose(2, 0, 1).reshape(128, 32)
    sp2[:, 48:80] = inp["ssm_norm_w"][0].reshape(32, 128).T
    sp2[:, 80:82] = inp["gdn_norm_w"][0].reshape(2, 128).T
    nwb = np.empty((128, 2, D_MODEL), np.float32)
    nwb[:, 0, :] = inp["ffn_norm_w"][0]
    nwb[:, 1, :] = inp["final_norm_w"]
    return {
        "wzg": tile_w(wz, 256),
        "wgt": tile_w(w_in[:, 22624:26720], 128),
        "wbs": tile_w(inp["w_branch_ssm"][0], 128),
        "wbg": tile_w(inp["w_branch_gdn"][0], 128),
        "wout": tile_w(inp["w_out"][0], 256),
        "wq": tile_w(inp["peer_w_q"][0], 512),
        "skT": np.ascontiguousarray(inp["peer_sub_keys"][0].reshape(16, 128, 128).transpose(2, 0, 1)),
        "u4": np.ascontiguousarray(inp["peer_u"][0].reshape(128, 128, 16, 128).transpose(0, 3, 2, 1)),
        "vtab": np.ascontiguousarray(inp["peer_v"][0]),
        "sp2": sp2,
        "nwb": nwb,
    }


def build_fused(T, neb=NEB):
    TL = T // NCORES
    nc = bass.Bass("TRN2", target_bir_lowering=False)
    D1 = p1_decl(nc, T)
    D2 = p2_decl(nc, TL, xtn="xTo")
    NGl = TL // TG
    ysend = [[nc.dram_tensor(f"ysend{j}_{h}", [NGl, 512, TG], BF16) for h in range(2)] for j in range(NCORES)]
    yrecv = nc.dram_tensor("yrecv", [NCORES, 2, NCORES, NGl, 512, TG], BF16)
    yown = nc.dram_tensor("yown", [2, NCORES, NGl, 512, TG], BF16)
    with ExitStack() as es:
        C = Ctx(nc, es)
        S = C.S
        K = build_consts(C)
        csem = es.enter_context(nc.semaphore("csem"))
        ccount = [0]
        tpd = TL // TT

        def store(tt, ys, ysk):
            j, off = tt // tpd, (tt % tpd) * TT
            for h in range(2):
                dst = ysend[j][h].ap()[off // TG].rearrange("(b p) t -> p b t", p=128)[:, :, off % TG:off % TG + TT]
                S.dma('sp', dst, ys[:, 4 * h:4 * h + 4, :], reads=[ysk], writes=[f'ysd{j}_{h}'])
            if tt % tpd == tpd - 1:
                for h in range(2):
                    S._deps('pool', [f'ysd{j}_{h}'], [f'yrc{j}_{h}'])
                    ins = nc.gpsimd.collective_compute("AllGather", ALU.bypass, replica_groups=[list(range(NCORES))],
                                                       ins=[ysend[j][h].ap().rearrange("g c t -> (g c) t").opt()],
                                                       outs=[yrecv.ap()[j, h].rearrange("r g c t -> (r g c) t").opt()])
                    ccount[0] += 1
                    ins.then_inc(csem, 1)
                    S._record((csem, ccount[0], ('cc', 0)), [f'ysd{j}_{h}'], [f'yrc{j}_{h}'])

        C.push()
        p1_body(C, K, T, D1, store)
        C.pop()
        for eng in ['sp', 'pool', 'act']:
            S._wait(eng, (csem, ccount[0], ('cc', 0)))
        pid = nc.sync.partition_id()
        src = yrecv.ap().rearrange("j h r g c t -> j (h r g c t)").rearrange("j (a n) -> j a n", a=16)[bass.ds(pid, 1)]
        S.dma('sp', yown.ap().rearrange("h r g c t -> (h r g c t)").rearrange("(a n) -> a n", a=16),
              src.rearrange("o a n -> (o a) n"), writes=['yown'])

        def yload(a, b, tsl):
            h = 0 if a < 32 else 1
            r = (a - 32 * h) // 4
            assert b - a == 4 and (a - 32 * h) % 4 == 0
            return yown.ap()[h, r, tsl.start // TG].rearrange("(b p) t -> p b t", p=128)

        p2_body(C, K, TL, D2, yload, neb)
        S.barrier()
    return nc


def fused_maps(inp, T):
    TL = T // NCORES
    x = inp["x"][0][:T]
    xT = np.ascontiguousarray(x.T)
    shared = prep_p2_shared(inp)
    maps = []
    for g in range(NCORES):
        ts = slice(g * TL, (g + 1) * TL)
        m = dict(shared)
        m.update(prep_p1(inp, g, T))
        m["xT"] = xT
        m["xTo"] = np.ascontiguousarray(xT[:, ts])
        m["xtok"] = np.ascontiguousarray(x[ts])
        maps.append(m)
    return maps


_CACHE = {}


def _get(name, fn):
    if name not in _CACHE:
        _CACHE[name] = fn()
    return _CACHE[name]


def kernel(**inp):
    inp = {k: np.asarray(v) for k, v in inp.items()}
    T = SEQ
    nc = _get(("fused", T), lambda: build_fused(T))
    maps = fused_maps(inp, T)
    r = run_bass_kernel_spmd(nc, maps, core_ids=list(range(NCORES)))
    out = np.concatenate([np.asarray(r.results[j]["out"]) for j in range(NCORES)], axis=0)
    return out.reshape(1, T, D_MODEL).astype(np.float32)
```

```python
import numpy as np
from contextlib import ExitStack
import concourse.bass as bass
import concourse.mybir as mybir
from concourse.bass_utils import run_bass_kernel_spmd

F32 = mybir.dt.float32
BF16 = mybir.dt.bfloat16
U32 = mybir.dt.uint32
AF = mybir.ActivationFunctionType
ALU = mybir.AluOpType
AX = mybir.AxisListType

D_MODEL = 2048
SEQ = 16384
NCORES = 8
EPS = 1e-6
NEG = -30000.0
SSM_PROJ = 10304
GDN_PROJ = 12320


import threading


class Coop:
    def __init__(self):
        self.active = False

    def run(self, fns, weights):
        n = len(fns)
        if n == 1:
            fns[0]()
            return
        self.sems = [threading.Semaphore(0) for _ in range(n)]
        self.done = [False] * n
        self.w = list(weights)
        self.left = list(weights)
        self.exc = None
        self.main = threading.Semaphore(0)
        self.active = True

        def worker(i, fn):
            self.sems[i].acquire()
            try:
                fn()
            except BaseException as e:
                self.exc = e
            self.done[i] = True
            self._pass(i, finishing=True)

        ths = [threading.Thread(target=worker, args=(i, f)) for i, f in enumerate(fns)]
        for t in ths:
            t.start()
        self.cur = 0
        self.sems[0].release()
        self.main.acquire()
        for t in ths:
            t.join()
        self.active = False
        if self.exc is not None:
            raise self.exc

    def _pass(self, i, finishing=False):
        n = len(self.done)
        for d in range(1, n + 1):
            j = (i + d) % n
            if not self.done[j]:
                if j == i:
                    return
                self.cur = j
                self.left[j] = self.w[j]
                self.sems[j].release()
                if not finishing:
                    self.sems[i].acquire()
                return
        if finishing:
            self.main.release()

    def switch(self):
        if not self.active:
            return
        i = self.cur
        self.left[i] -= 1
        if self.left[i] > 0:
            return
        self._pass(i)


class Sched:
    NDMA = 12
    LIMIT = 30000

    def __init__(self, nc, es):
        self.nc = nc
        self.es = es
        self.E = dict(pe=nc.tensor, act=nc.scalar, dve=nc.vector, pool=nc.gpsimd, sp=nc.sync)
        self.sem = {}
        self.cnt = {}
        self.gen = {}
        for k in ['pe', 'act', 'dve', 'pool']:
            self._newsem(k)
        self.dq = {q: dict(sem=[None] * self.NDMA, use=[0] * self.NDMA, gen=[0] * self.NDMA, nxt=0) for q in ['sp', 'pool', 'act']}
        self.lastw = {}
        self.reads = {}
        self.waited = {}
        self.bank_last = {}
        self.coop = Coop()
        self.nwaits = 0
        self.nops = 0

    def _newsem(self, k):
        g = self.gen.get(k, -1) + 1
        self.gen[k] = g
        self.sem[k] = self.es.enter_context(self.nc.semaphore(f"s_{k}{g}"))
        self.cnt[k] = 0

    def _wait(self, eng, ev):
        sem, val, sid = ev
        if self.waited.get((eng, sid), 0) >= val:
            return
        self.E[eng].wait_ge(sem, val)
        self.nwaits += 1
        self.waited[(eng, sid)] = val

    def _deps(self, eng, reads, writes):
        best = {}

        def add(e):
            if e is None:
                return
            if e[2] not in best or best[e[2]][1] < e[1]:
                best[e[2]] = e
        for k in list(reads) + list(writes):
            add(self.lastw.get(k))
        for k in writes:
            for e in self.reads.get(k, {}).values():
                add(e)
        for k in list(reads) + list(writes):
            if len(k) > 1 and k[0] == 'P' and k[1].isdigit():
                for sid, e in self.bank_last.get(k[1], {}).items():
                    if sid[0] != eng:
                        add(e)
        for e in best.values():
            if eng == 'pe' and e[2][0] == 'pe':
                continue
            self._wait(eng, e)

    def _record(self, ev, reads, writes):
        for k in list(reads) + list(writes):
            if len(k) > 1 and k[0] == 'P' and k[1].isdigit():
                self.bank_last.setdefault(k[1], {})[ev[2]] = ev
        for k in writes:
            self.lastw[k] = ev
            self.reads[k] = {}
        for k in reads:
            d = self.reads.setdefault(k, {})
            d[ev[2]] = ev

    def op(self, eng, fn, reads=(), writes=()):
        if self.cnt[eng] >= self.LIMIT:
            self._newsem(eng)
        self._deps(eng, reads, writes)
        ins = fn(self.E[eng])
        self.cnt[eng] += 1
        self.nops += 1
        ins.then_inc(self.sem[eng], 1)
        ev = (self.sem[eng], self.cnt[eng], (eng, self.gen[eng]))
        self._record(ev, reads, writes)
        self.coop.switch()
        return ev

    def dma(self, q, out, in_, reads=(), writes=(), **kw):
        D = self.dq[q]
        i = D['nxt']
        D['nxt'] = (i + 1) % self.NDMA
        if D['sem'][i] is None or 16 * (D['use'][i] + 1) > self.LIMIT:
            if D['sem'][i] is not None:
                self._wait(q, (D['sem'][i], 16 * D['use'][i], ('d', q, i, D['gen'][i])))
            D['gen'][i] += 1
            D['sem'][i] = self.es.enter_context(self.nc.semaphore(f"d{q}_{i}_{D['gen'][i]}"))
            D['use'][i] = 0
        sid = ('d', q, i, D['gen'][i])
        if D['use'][i] > 0:
            self._wait(q, (D['sem'][i], 16 * D['use'][i], sid))
        self._deps(q, reads, writes)
        D['use'][i] += 1
        ins = self.E[q].dma_start(out=out, in_=in_, **kw)
        ins.then_inc(D['sem'][i], 16)
        ev = (D['sem'][i], 16 * D['use'][i], sid)
        self._record(ev, reads, writes)
        self.coop.switch()
        return ev

    def barrier(self):
        for eng in ['pe', 'act', 'dve', 'pool', 'sp']:
            for other in ['pe', 'act', 'dve', 'pool']:
                if other != eng and self.cnt[other] > 0:
                    self._wait(eng, (self.sem[other], self.cnt[other], (other, self.gen[other])))
            for q, D in self.dq.items():
                for i in range(self.NDMA):
                    if D['sem'][i] is not None and D['use'][i] > 0:
                        self._wait(eng, (D['sem'][i], 16 * D['use'][i], ('d', q, i, D['gen'][i])))

    def wait_all(self, eng):
        best = {}
        for e in self.lastw.values():
            if e[2] not in best or best[e[2]][1] < e[1]:
                best[e[2]] = e
        for e in best.values():
            self._wait(eng, e)


def bc(ap, axis, n):
    dims = [list(d) for d in ap.ap]
    dims.insert(axis, [0, n])
    return bass.AP(ap.tensor, ap.offset, dims)


def kq(b):
    return [f'P{b}q{j}' for j in range(4)]


class Ctx:
    def __init__(self, nc, es):
        self.nc = nc
        self.es = es
        self.S = Sched(nc, es)
        self.scopes = []
        self.uid = 0

    def sb(self, name, shape, dt):
        es = self.scopes[-1] if self.scopes else self.es
        if self.scopes:
            self.uid += 1
            name = f"{name}_u{self.uid}"
        return es.enter_context(self.nc.sbuf_tensor(name, list(shape), dt))

    def push(self):
        sc = ExitStack()
        self.scopes.append(sc)
        return sc

    def pop(self):
        self.S.barrier()
        self.scopes.pop().close()

    def ps(self, name, shape=(128, 512), dt=F32):
        es = self.scopes[-1] if self.scopes else self.es
        if self.scopes:
            self.uid += 1
            name = f"{name}_u{self.uid}"
        return es.enter_context(self.nc.psum_tensor(name, list(shape), dt))


def build_consts(C):
    S, nc = C.S, C.nc
    K = {}
    ones_f = C.sb("ones_f", [128, 128], F32)
    zero_f = C.sb("zero_f", [128, 128], F32)
    S.op('pool', lambda e: e.memset(ones_f[:], 1.0), writes=['ones_f'])
    S.op('pool', lambda e: e.memset(zero_f[:], 0.0), writes=['zero_f'])

    def sel(name, src, srckey, base, cm, step, fill):
        t = C.sb(name, [128, 128], F32)
        S.op('pool', lambda e: e.affine_select(out=t[:], in_=src[:], pattern=[[step, 128]],
                                               compare_op=ALU.is_ge, fill=fill, base=base, channel_multiplier=cm),
             reads=[srckey], writes=[name])
        return t
    tmp = sel("tmp_ge", ones_f, 'ones_f', 0, -1, 1, 0.0)
    ident_f = sel("ident_f", tmp, 'tmp_ge', 0, 1, -1, 0.0)
    tri_f = tmp
    negmask = sel("negmask", zero_f, 'zero_f', 0, -1, 1, NEG)
    negmask_s = sel("negmask_s", zero_f, 'zero_f', -1, -1, 1, NEG)
    tribd = C.sb("tribd", [128, 128], F32)
    S.op('pool', lambda e: e.tensor_copy(out=tribd[:], in_=tri_f[:]), reads=['tmp_ge'], writes=['tribd'])
    S.op('pool', lambda e: e.memset(tribd[0:64, 64:128], 0.0), writes=['tribd'])
    nm_bd = C.sb("nm_bd", [128, 128], F32)
    S.op('pool', lambda e: e.tensor_copy(out=nm_bd[:], in_=negmask[:]), reads=['negmask'], writes=['nm_bd'])
    S.op('pool', lambda e: e.memset(nm_bd[0:64, 64:128], NEG), writes=['nm_bd'])
    nm_bds = C.sb("nm_bds", [128, 128], F32)
    S.op('pool', lambda e: e.tensor_copy(out=nm_bds[:], in_=negmask_s[:]), reads=['negmask_s'], writes=['nm_bds'])
    S.op('pool', lambda e: e.memset(nm_bds[0:64, 64:128], NEG), writes=['nm_bds'])
    selA = C.sb("selA", [128, 128], F32)
    selB = C.sb("selB", [128, 128], F32)
    blk1 = C.sb("blk1", [128, 128], F32)
    S.op('pool', lambda e: e.memset(selA[:], 0.0), writes=['selA'])
    S.op('pool', lambda e: e.memset(selA[0:64, :], 1.0), writes=['selA'])
    S.op('pool', lambda e: e.memset(selB[:], 0.0), writes=['selB'])
    S.op('pool', lambda e: e.memset(selB[64:128, :], 1.0), writes=['selB'])
    S.op('pool', lambda e: e.memset(blk1[:], 0.0), writes=['blk1'])
    S.op('pool', lambda e: e.memset(blk1[0:64, 0:64], 1.0), writes=['blk1'])
    S.op('pool', lambda e: e.memset(blk1[64:128, 64:128], 1.0), writes=['blk1'])
    ident_b = C.sb("ident_b", [128, 128], BF16)
    ones_b = C.sb("ones_b", [128, 128], BF16)
    S.op('pool', lambda e: e.tensor_copy(out=ident_b[:], in_=ident_f[:]), reads=['ident_f'], writes=['ident_b'])
    S.op('pool', lambda e: e.tensor_copy(out=ones_b[:], in_=ones_f[:]), reads=['ones_f'], writes=['ones_b'])
    eps_t = C.sb("eps_t", [128, 1], F32)
    one_t = C.sb("one_t", [128, 1], F32)
    S.op('pool', lambda e: e.memset(eps_t[:], EPS), writes=['eps_t'])
    S.op('pool', lambda e: e.memset(one_t[:], 1.0), writes=['one_t'])
    K.update(eps_t=eps_t, one_t=one_t)
    K.update(ones_f=ones_f, zero_f=zero_f, ident_f=ident_f, tri_f=tri_f, negmask=negmask, negmask_s=negmask_s,
             tribd=tribd, nm_bd=nm_bd, nm_bds=nm_bds, selA=selA, selB=selB, blk1=blk1, ident_b=ident_b, ones_b=ones_b)
    return K


class QAlloc:
    def __init__(self, P, banks):
        self.P = P
        self.banks = banks
        self.i = [0 for _ in banks]

    def _next(self, hh):
        b = self.banks[hh][self.i[hh] % len(self.banks[hh])]
        self.i[hh] += 1
        return b

    def Q(self, hh=0):
        b = self._next(hh)
        return self.P[b][:, 0:128], [f'P{b}q0']

    def H(self, hh=0):
        b = self._next(hh)
        return self.P[b][:, 0:256], [f'P{b}q0', f'P{b}q1']


def gdn_pair(C, K, P, QA, so, par, ys, ysk, sub, g_t, beta_t, lnb_t, Sg_f, Sg_b, gsq, rn, kn, qn, qd, kb, kdec, vb, gsc,
             Xg, Xg2, Eg, Ee, Es, attnT, Mm, Lm, Tm, Ttm, Pm, Ptm, Tb, nwT, vnb, dcb):
    S = C.S
    ones_f, ident_f, ones_b, ident_b = K['ones_f'], K['ident_f'], K['ones_b'], K['ident_b']
    cs = slice(sub * 128, (sub + 1) * 128)
    HH = range(2)
    evi = [0]

    def evac(out, in_, reads, writes):
        evi[0] += 1
        if evi[0] % 2:
            S.op('act', lambda e: e.copy(out=out, in_=in_), reads=reads, writes=writes)
        else:
            S.op('dve', lambda e: e.tensor_copy(out=out, in_=in_), reads=reads, writes=writes)

    for hh in HH:
        S.op('act', lambda e, hh=hh: e.activation(out=gsq[hh][:, 0, :], in_=so[:, 8 + hh, cs], func=AF.Square),
             reads=[f'so{par}_{8 + hh}'], writes=[f'gsq{hh}'])
        S.op('act', lambda e, hh=hh: e.activation(out=gsq[hh][:, 1, :], in_=so[:, 6 + hh, cs], func=AF.Square),
             reads=[f'so{par}_{6 + hh}'], writes=[f'gsq{hh}'])
        hq_, hk = QA.H(hh)
        S.op('pe', lambda e, hh=hh, hq_=hq_: e.matmul(hq_, lhsT=ones_b[:], rhs=gsq[hh][:].rearrange("p a t -> p (a t)"), start=True, stop=True),
             reads=['ones_b', f'gsq{hh}'], writes=hk)
        S.op('act', lambda e, hh=hh, hq_=hq_: e.activation(out=rn[hh][:].rearrange("p a t -> p (a t)"), in_=hq_, func=AF.Ln, bias=K['eps_t'][:]),
             reads=hk + ['eps_t'], writes=[f'rn{hh}'])
        S.op('act', lambda e, hh=hh: e.activation(out=rn[hh][:], in_=rn[hh][:], func=AF.Exp, scale=-0.5), reads=[f'rn{hh}'], writes=[f'rn{hh}'])
        S.op('dve', lambda e, hh=hh: e.tensor_tensor(out=kn[hh][:], in0=so[:, 8 + hh, cs], in1=rn[hh][:, 0, :], op=ALU.mult),
             reads=[f'so{par}_{8 + hh}', f'rn{hh}'], writes=[f'kn{hh}'])
        S.op('dve', lambda e, hh=hh: e.scalar_tensor_tensor(out=qn[hh][:], in0=so[:, 6 + hh, cs], scalar=128.0 ** -0.5, in1=rn[hh][:, 1, :],
                                                           op0=ALU.mult, op1=ALU.mult),
             reads=[f'so{par}_{6 + hh}', f'rn{hh}'], writes=[f'qn{hh}'])
    q7, q7k = QA.Q(0)
    S.op('pe', lambda e: e.matmul(q7[:, 0:2], lhsT=K['tribd'][:], rhs=g_t[:, sub, :], start=True, stop=True), reads=['tribd', f'g_t{par}'], writes=q7k)
    S.op('pe', lambda e: e.matmul(q7[:, 2:4], lhsT=K['blk1'][:], rhs=g_t[:, sub, :], start=True, stop=True), reads=['blk1', f'g_t{par}'], writes=q7k)
    S.op('pe', lambda e: e.matmul(q7[:, 4:6], lhsT=K['selA'][:], rhs=g_t[:, sub, :], start=True, stop=True), reads=['selA', f'g_t{par}'], writes=q7k)
    S.op('pe', lambda e: e.matmul(q7[:, 6:8], lhsT=K['selB'][:], rhs=g_t[:, sub, :], start=True, stop=True), reads=['selB', f'g_t{par}'], writes=q7k)
    for hh in HH:
        gk = f'gsc{hh}'
        S.op('dve', lambda e, hh=hh: e.tensor_copy(out=gsc[hh][:, 0:1], in_=q7[:, hh:hh + 1]), reads=q7k, writes=[gk])
        S.op('dve', lambda e, hh=hh: e.tensor_scalar(out=gsc[hh][:, 1:2], in0=q7[:, hh:hh + 1], scalar1=-1.0, scalar2=None, op0=ALU.mult),
             reads=q7k, writes=[gk])
        S.op('dve', lambda e, hh=hh: e.tensor_tensor(out=gsc[hh][:, 4:5], in0=q7[:, 2 + hh:3 + hh], in1=q7[:, hh:hh + 1], op=ALU.subtract) if False else
             e.tensor_tensor(out=gsc[hh][:, 4:5], in0=q7[:, 2 + hh:3 + hh], in1=gsc[hh][:, 0:1], op=ALU.subtract),
             reads=q7k + [gk], writes=[gk])
        S.op('act', lambda e, hh=hh: e.activation(out=gsc[hh][:, 4:5], in_=gsc[hh][:, 4:5], func=AF.Exp), reads=[gk], writes=[gk])
        S.op('act', lambda e, hh=hh: e.activation(out=gsc[hh][:, 3:4], in_=gsc[hh][:, 0:1], func=AF.Exp), reads=[gk], writes=[gk])
        S.op('dve', lambda e, hh=hh: e.tensor_tensor(out=gsc[hh][:, 3:4], in0=gsc[hh][:, 3:4], in1=beta_t[:, sub, hh:hh + 1], op=ALU.mult),
             reads=[gk, f'beta_t{par}'], writes=[gk])
        S.op('act', lambda e, hh=hh: e.activation(out=dcb[hh][:, 0:1], in_=q7[:, 4 + hh:5 + hh], func=AF.Exp), reads=q7k, writes=[f'dcb{hh}'])
        S.op('act', lambda e, hh=hh: e.activation(out=dcb[hh][:, 1:2], in_=q7[:, 6 + hh:7 + hh], func=AF.Exp), reads=q7k, writes=[f'dcb{hh}'])
    for hh in HH:
        qk_, qkk = QA.Q(hh)
        S.op('pe', lambda e, hh=hh, qk_=qk_: e.matmul(qk_, lhsT=kn[hh][:], rhs=ident_b[:], start=True, stop=True),
             reads=[f'kn{hh}', 'ident_b'], writes=qkk)
        S.op('dve', lambda e, hh=hh, qk_=qk_: e.tensor_scalar(out=kb[hh][:], in0=qk_, scalar1=gsc[hh][:, 3:4], scalar2=None, op0=ALU.mult),
             reads=qkk + [f'gsc{hh}'], writes=[f'kb{hh}'])
        S.op('dve', lambda e, hh=hh, qk_=qk_: e.tensor_scalar(out=kdec[hh][:], in0=qk_, scalar1=gsc[hh][:, 4:5], scalar2=None, op0=ALU.mult),
             reads=qkk + [f'gsc{hh}'], writes=[f'kdec{hh}'])
        hv, hvk = QA.H(hh)
        for vbk in range(2):
            S.op('pe', lambda e, hh=hh, vbk=vbk, hv=hv: e.transpose(hv[:, vbk * 128:(vbk + 1) * 128], so[:, 10 + 2 * hh + vbk, cs], ident_f[:]),
                 reads=[f'so{par}_{10 + 2 * hh + vbk}', 'ident_f'], writes=[hvk[vbk]])
        S.op('dve', lambda e, hh=hh, hv=hv: e.tensor_scalar(out=vb[hh][:], in0=hv, scalar1=beta_t[:, sub, hh:hh + 1], scalar2=None, op0=ALU.mult),
             reads=hvk + [f'beta_t{par}'], writes=[f'vb{hh}'])
    for hh in HH:
        S.op('dve', lambda e, hh=hh: e.tensor_scalar(out=Xg[hh][:], in0=K['tribd'][:], scalar1=g_t[:, sub, hh:hh + 1], scalar2=None, op0=ALU.mult),
             reads=['tribd', f'g_t{par}'], writes=[f'Xg{hh}'])
        S.op('dve', lambda e, hh=hh: e.scalar_tensor_tensor(out=Xg2[hh][:], in0=ident_f[:], scalar=lnb_t[:, sub, hh:hh + 1], in1=Xg[hh][:],
                                                           op0=ALU.mult, op1=ALU.add),
             reads=['ident_f', f'lnb_t{par}', f'Xg{hh}'], writes=[f'Xg2{hh}'])
        a, ak = QA.Q(hh)
        S.op('pe', lambda e, hh=hh, a=a: e.matmul(a, lhsT=ones_f[:], rhs=Xg[hh][:], start=True, stop=False), reads=['ones_f', f'Xg{hh}'], writes=ak)
        S.op('pe', lambda e, hh=hh, a=a: e.matmul(a, lhsT=ident_f[:], rhs=K['nm_bd'][:], start=False, stop=True), reads=['ident_f', 'nm_bd'], writes=ak)
        S.op('act', lambda e, hh=hh, a=a: e.activation(out=Ee[hh][:], in_=a, func=AF.Exp, bias=gsc[hh][:, 1:2]), reads=ak + [f'gsc{hh}'], writes=[f'Ee{hh}'])
        b_, bk = QA.Q(hh)
        S.op('pe', lambda e, hh=hh, b_=b_: e.matmul(b_, lhsT=ones_f[:], rhs=Xg2[hh][:], start=True, stop=False), reads=['ones_f', f'Xg2{hh}'], writes=bk)
        S.op('pe', lambda e, hh=hh, b_=b_: e.matmul(b_, lhsT=ident_f[:], rhs=K['nm_bds'][:], start=False, stop=True), reads=['ident_f', 'nm_bds'], writes=bk)
        S.op('act', lambda e, hh=hh, b_=b_: e.activation(out=Es[hh][:], in_=b_, func=AF.Exp, bias=gsc[hh][:, 1:2]), reads=bk + [f'gsc{hh}'], writes=[f'Es{hh}'])
        c_, ck = QA.Q(hh)
        S.op('pe', lambda e, hh=hh, c_=c_: e.matmul(c_, lhsT=ones_f[:], rhs=Xg[hh][:], start=True, stop=True), reads=['ones_f', f'Xg{hh}'], writes=ck)
        S.op('act', lambda e, hh=hh, c_=c_: e.activation(out=Eg[hh][:], in_=c_, func=AF.Exp), reads=ck, writes=[f'Eg{hh}'])
        S.op('dve', lambda e, hh=hh: e.tensor_tensor(out=qd[hh][:], in0=qn[hh][:], in1=Eg[hh][:], op=ALU.mult),
             reads=[f'qn{hh}', f'Eg{hh}'], writes=[f'qd{hh}'])
        d_, dk_ = QA.Q(hh)
        S.op('pe', lambda e, hh=hh, d_=d_: e.matmul(d_, lhsT=kn[hh][:], rhs=kn[hh][:], start=True, stop=True), reads=[f'kn{hh}'], writes=dk_)
        S.op('dve', lambda e, hh=hh, d_=d_: e.tensor_tensor(out=Mm[hh][:], in0=d_, in1=Es[hh][:], op=ALU.mult), reads=dk_ + [f'Es{hh}'], writes=[f'Mm{hh}'])
        e_, ek = QA.Q(hh)
        S.op('pe', lambda e, hh=hh, e_=e_: e.matmul(e_, lhsT=kn[hh][:], rhs=qn[hh][:], start=True, stop=True), reads=[f'kn{hh}', f'qn{hh}'], writes=ek)
        S.op('dve', lambda e, hh=hh, e_=e_: e.tensor_tensor(out=attnT[hh][:], in0=e_, in1=Ee[hh][:], op=ALU.mult), reads=ek + [f'Ee{hh}'], writes=[f'attnT{hh}'])
    for hh in HH:
        a, ak = QA.Q(hh)
        S.op('pe', lambda e, hh=hh, a=a: e.transpose(a, Mm[hh][:], ident_f[:]), reads=[f'Mm{hh}', 'ident_f'], writes=ak)
        evac(Lm[hh][:], a, ak, [f'Lm{hh}'])
        S.op('dve', lambda e, hh=hh: e.tensor_tensor(out=Tm[hh][0][:], in0=ident_f[:], in1=Mm[hh][:], op=ALU.subtract),
             reads=['ident_f', f'Mm{hh}'], writes=[f'Tm{hh}_0'])
        S.op('pool', lambda e, hh=hh: e.tensor_tensor(out=Ttm[hh][0][:], in0=ident_f[:], in1=Lm[hh][:], op=ALU.subtract),
             reads=['ident_f', f'Lm{hh}'], writes=[f'Ttm{hh}_0'])
        a, ak = QA.Q(hh)
        S.op('pe', lambda e, hh=hh, a=a: e.matmul(a, lhsT=Lm[hh][:], rhs=Mm[hh][:], start=True, stop=True), reads=[f'Lm{hh}', f'Mm{hh}'], writes=ak)
        evac(Pm[hh][0][:], a, ak, [f'Pm{hh}_0'])
        a, ak = QA.Q(hh)
        S.op('pe', lambda e, hh=hh, a=a: e.matmul(a, lhsT=Mm[hh][:], rhs=Lm[hh][:], start=True, stop=True), reads=[f'Lm{hh}', f'Mm{hh}'], writes=ak)
        evac(Ptm[hh][0][:], a, ak, [f'Ptm{hh}_0'])
    for k in range(1, 6):
        cur, nxt = (k - 1) % 2, k % 2
        pc, pn = (k - 1) % 2, k % 2
        for hh in HH:
            a, ak = QA.Q(hh)
            S.op('pe', lambda e, hh=hh, a=a: e.matmul(a, lhsT=ident_f[:], rhs=Tm[hh][cur][:], start=True, stop=False),
                 reads=['ident_f', f'Tm{hh}_{cur}'], writes=ak)
            S.op('pe', lambda e, hh=hh, a=a: e.matmul(a, lhsT=Ttm[hh][cur][:], rhs=Pm[hh][pc][:], start=False, stop=True),
                 reads=[f'Ttm{hh}_{cur}', f'Pm{hh}_{pc}'], writes=ak)
            evac(Tm[hh][nxt][:], a, ak, [f'Tm{hh}_{nxt}'])
            if k < 5:
                a, ak = QA.Q(hh)
                S.op('pe', lambda e, hh=hh, a=a: e.matmul(a, lhsT=ident_f[:], rhs=Ttm[hh][cur][:], start=True, stop=False),
                     reads=['ident_f', f'Ttm{hh}_{cur}'], writes=ak)
                S.op('pe', lambda e, hh=hh, a=a: e.matmul(a, lhsT=Pm[hh][pc][:], rhs=Ttm[hh][cur][:], start=False, stop=True),
                     reads=[f'Ttm{hh}_{cur}', f'Pm{hh}_{pc}'], writes=ak)
                evac(Ttm[hh][nxt][:], a, ak, [f'Ttm{hh}_{nxt}'])
                a, ak = QA.Q(hh)
                S.op('pe', lambda e, hh=hh, a=a: e.matmul(a, lhsT=Ptm[hh][pc][:], rhs=Pm[hh][pc][:], start=True, stop=True),
                     reads=[f'Ptm{hh}_{pc}', f'Pm{hh}_{pc}'], writes=ak)
                evac(Pm[hh][pn][:], a, ak, [f'Pm{hh}_{pn}'])
                a, ak = QA.Q(hh)
                S.op('pe', lambda e, hh=hh, a=a: e.matmul(a, lhsT=Pm[hh][pc][:], rhs=Ptm[hh][pc][:], start=True, stop=True),
                     reads=[f'Ptm{hh}_{pc}', f'Pm{hh}_{pc}'], writes=ak)
                evac(Ptm[hh][pn][:], a, ak, [f'Ptm{hh}_{pn}'])
    for hh in HH:
        S.op('act', lambda e, hh=hh: e.copy(out=Tb[hh][:], in_=Tm[hh][1][:]), reads=[f'Tm{hh}_1'], writes=[f'Tb{hh}'])
        a, ak = QA.Q(hh)
        S.op('pe', lambda e, hh=hh, a=a: e.matmul(a, lhsT=kb[hh][:], rhs=Tb[hh][:], start=True, stop=True), reads=[f'kb{hh}', f'Tb{hh}'], writes=ak)
        S.op('act', lambda e, hh=hh, a=a: e.mul(out=nwT[hh][:], in_=a, mul=-1.0), reads=ak, writes=[f'nwT{hh}'])
    for c in range(2):
        jr = slice(c * 64, (c + 1) * 64)
        for hh in HH:
            hv, hvk = QA.H(hh)
            S.op('pe', lambda e, hh=hh, hv=hv: e.matmul(hv[jr, :], lhsT=Tb[hh][jr, jr], rhs=vb[hh][jr, :], start=True, stop=False),
                 reads=[f'Tb{hh}', f'vb{hh}'], writes=hvk)
            S.op('pe', lambda e, hh=hh, hv=hv: e.matmul(hv[jr, :], lhsT=nwT[hh][:, jr], rhs=Sg_b[hh][:], start=False, stop=True),
                 reads=[f'nwT{hh}', f'Sg_b{hh}'], writes=hvk)
            evac(vnb[hh][jr, :], hv[jr, :], hvk, [f'vnb{hh}'])
        for hh in HH:
            for dvb in range(2):
                a, ak = QA.Q(hh)
                S.op('pe', lambda e, hh=hh, dvb=dvb, a=a: e.matmul(a[:, 0:64], lhsT=Sg_b[hh][:, dvb * 128:(dvb + 1) * 128], rhs=qd[hh][:, jr],
                                                                 start=True, stop=False), reads=[f'Sg_b{hh}', f'qd{hh}'], writes=ak)
                S.op('pe', lambda e, hh=hh, dvb=dvb, a=a: e.matmul(a[:, 0:64], lhsT=vnb[hh][jr, dvb * 128:(dvb + 1) * 128], rhs=attnT[hh][jr, jr],
                                                                 start=False, stop=True), reads=[f'vnb{hh}', f'attnT{hh}'], writes=ak)
                evac(ys[:, 4 + 2 * hh + dvb, sub * 128 + c * 64:sub * 128 + c * 64 + 64], a[:, 0:64], ak, [ysk])
        for hh in HH:
            hv, hvk = QA.H(hh)
            S.op('pe', lambda e, hh=hh, hv=hv: e.matmul(hv, lhsT=kdec[hh][jr, :], rhs=vnb[hh][jr, :], start=True, stop=True),
                 reads=[f'kdec{hh}', f'vnb{hh}'], writes=hvk)
            S.op('dve', lambda e, hh=hh, hv=hv: e.scalar_tensor_tensor(out=Sg_f[hh][:], in0=Sg_f[hh][:], scalar=dcb[hh][:, c:c + 1], in1=hv,
                                                                     op0=ALU.mult, op1=ALU.add),
                 reads=[f'Sg_f{hh}', f'dcb{hh}'] + hvk, writes=[f'Sg_f{hh}'])
            S.op('act', lambda e, hh=hh: e.copy(out=Sg_b[hh][:], in_=Sg_f[hh][:]), reads=[f'Sg_f{hh}'], writes=[f'Sg_b{hh}'])

NF = 14
NCOL1 = NF * 128 + 16
TT = 256


def p1_decl(nc, T):
    return dict(
        xT=nc.dram_tensor("xT", [D_MODEL, T], F32, kind="ExternalInput").ap(),
        w1=nc.dram_tensor("w1", [D_MODEL, NCOL1], F32, kind="ExternalInput").ap(),
        mixw=nc.dram_tensor("mixw", [128, 16], F32, kind="ExternalInput").ap(),
        convw=nc.dram_tensor("convw", [128, NF, 4], F32, kind="ExternalInput").ap(),
        convb=nc.dram_tensor("convb", [128, NF], F32, kind="ExternalInput").ap(),
        hp=nc.dram_tensor("hp", [128, 32], F32, kind="ExternalInput").ap())


def p1_body(C, K, T, D, store):
    S = C.S
    NTT = T // TT
    xT_d, w1_d, mixw_d, convw_d, convb_d, hp_d = D['xT'], D['w1'], D['mixw'], D['convw'], D['convb'], D['hp']
    ones_f, ident_f, tri_f = K['ones_f'], K['ident_f'], K['tri_f']
    ones_b, ident_b = K['ones_b'], K['ident_b']
    W = C.sb("W", [128, 16, NCOL1], BF16)
    mixw = C.sb("mixw_s", [128, 16], F32)
    convw = C.sb("convw_s", [128, NF, 4], F32)
    convb = C.sb("convb_s", [128, NF], F32)
    hp = C.sb("hp_s", [128, 32], F32)
    hq = C.sb("hq", [128, 16], F32)
    S.dma('sp', mixw[:], mixw_d, writes=['mixw'])
    S.dma('sp', convw[:], convw_d, writes=['convw'])
    S.dma('sp', convb[:], convb_d, writes=['convb'])
    S.dma('sp', hp[:], hp_d, writes=['hp'])
    S.op('act', lambda e: e.activation(out=hq[:, 0:8], in_=hp[:, 8:16], func=AF.Exp), reads=['hp'], writes=['hq'])
    S.op('act', lambda e: e.activation(out=hq[:, 8:10], in_=hp[:, 22:24], func=AF.Exp), reads=['hp'], writes=['hq'])
    S.op('dve', lambda e: e.tensor_scalar(out=hq[:, 0:10], in0=hq[:, 0:10], scalar1=-1.0, scalar2=None, op0=ALU.mult),
         reads=['hq'], writes=['hq'])
    w1v = w1_d.rearrange("(k p) c -> p k c", p=128)
    for k in range(16):
        S.dma('pool', W[:, k, :], w1v[:, k, :], writes=[f'W{k}'])
    xs = [C.sb(f"xs{i}", [128, 4, TT], F32) for i in range(2)]
    xsq = [C.sb(f"xsq{i}", [128, 4, TT], BF16) for i in range(2)]
    xn = [C.sb(f"xn{i}", [128, 16, TT], BF16) for i in range(2)]
    rb = C.sb("rb", [128, TT], F32)
    convin = C.sb("convin", [128, NF, 3 + TT], F32)
    cacc = [C.sb(f"cacc{i}", [128, TT], F32) for i in range(2)]
    so2 = [C.sb(f"so_{i}", [128, NF, TT], F32) for i in range(2)]
    smf = C.sb("smf", [16, TT], F32)
    smt2 = [C.sb(f"smt_{i}", [128, 2, 16], F32) for i in range(2)]
    ystage = [C.sb(f"ystage{i}", [128, 8, TT], BF16) for i in range(2)]
    S.op('pool', lambda e: e.memset(convin[:], 0.0), writes=[f'convin{b}' for b in range(NF)])
    dt_t2 = [C.sb(f"dt_t_{i}", [128, 2, 8], F32) for i in range(2)]
    dA_t2 = [C.sb(f"dA_t_{i}", [128, 2, 8], F32) for i in range(2)]
    g_t2 = [C.sb(f"g_t_{i}", [128, 2, 2], F32) for i in range(2)]
    beta_t2 = [C.sb(f"beta_t_{i}", [128, 2, 2], F32) for i in range(2)]
    lnb_t2 = [C.sb(f"lnb_t_{i}", [128, 2, 2], F32) for i in range(2)]
    sm_tmp2 = [C.sb(f"sm_tmp_{i}", [128, 2, 16], F32) for i in range(2)]
    h_f = C.sb("h_f", [128, 8, 64], F32)
    h_b = C.sb("h_b", [128, 512], BF16)
    S.op('pool', lambda e: e.memset(h_f[:], 0.0), writes=['h_f'])
    S.op('pool', lambda e: e.memset(h_b[:], 0.0), writes=['h_b'])
    xtok = C.sb("xtok", [128, 8, 64], F32)
    xdt = C.sb("xdt", [128, 8, 64], BF16)
    xdtw = C.sb("xdtw", [128, 8, 64], BF16)
    btok = C.sb("btok", [128, 128], BF16)
    bfb = C.sb("bfb", [128, 128], BF16)
    cfb = C.sb("cfb", [128, 128], BF16)
    Xda = C.sb("Xda", [128, 8, 128], F32)
    acs = C.sb("acs", [128, 8], F32)
    nacs = C.sb("nacs", [128, 8], F32)
    tend = C.sb("tend", [128, 8], F32)
    cdec = C.sb("cdec", [128, 8], F32)
    wsc = C.sb("wsc", [128, 8], F32)
    decay = C.sb("decay", [128, 8, 128], BF16)
    Eb = C.sb("Eb", [128, 8, 128], F32)
    Cdec = C.sb("Cdec", [128, 8, 128], BF16)
    scor = C.sb("scor", [128, 8, 128], BF16)
    nmrep = C.sb("nmrep", [128, 8, 128], F32)
    for r in range(8):
        S.op('pool', lambda e, r=r: e.tensor_copy(out=nmrep[:, r, :], in_=K['negmask'][:]), reads=['negmask'], writes=['nmrep'])
    Sg_f = [C.sb(f"Sg_f{h}", [128, 256], F32) for h in range(2)]
    Sg_b = [C.sb(f"Sg_b{h}", [128, 256], BF16) for h in range(2)]
    for h in range(2):
        S.op('pool', lambda e, h=h: e.memset(Sg_f[h][:], 0.0), writes=[f'Sg_f{h}'])
        S.op('pool', lambda e, h=h: e.memset(Sg_b[h][:], 0.0), writes=[f'Sg_b{h}'])
    gsq = [C.sb(f"gsq{h}", [128, 2, 128], BF16) for h in range(2)]
    rn = [C.sb(f"rn{h}", [128, 2, 128], F32) for h in range(2)]
    kn = [C.sb(f"kn{h}", [128, 128], BF16) for h in range(2)]
    qn = [C.sb(f"qn{h}", [128, 128], BF16) for h in range(2)]
    qd = [C.sb(f"qd{h}", [128, 128], BF16) for h in range(2)]
    kb = [C.sb(f"kb{h}", [128, 128], BF16) for h in range(2)]
    kdec = [C.sb(f"kdec{h}", [128, 128], BF16) for h in range(2)]
    vb = [C.sb(f"vb{h}", [128, 256], BF16) for h in range(2)]
    gsc = [C.sb(f"gsc{h}", [128, 12], F32) for h in range(2)]
    Xg = [C.sb(f"Xg{h}", [128, 128], F32) for h in range(2)]
    Xg2 = [C.sb(f"Xg2{h}", [128, 128], F32) for h in range(2)]
    Eg = [C.sb(f"Eg{h}", [128, 128], F32) for h in range(2)]
    Ee = [C.sb(f"Ee{h}", [128, 128], F32) for h in range(2)]
    Es = [C.sb(f"Es{h}", [128, 128], F32) for h in range(2)]
    attnT = [C.sb(f"attnT{h}", [128, 128], BF16) for h in range(2)]
    Mm = [C.sb(f"Mm{h}", [128, 128], F32) for h in range(2)]
    Lm = [C.sb(f"Lm{h}", [128, 128], F32) for h in range(2)]
    Tm = [[C.sb(f"Tm{h}_{i}", [128, 128], F32) for i in range(2)] for h in range(2)]
    Ttm = [[C.sb(f"Ttm{h}_{i}", [128, 128], F32) for i in range(2)] for h in range(2)]
    Pm = [[C.sb(f"Pm{h}_{i}", [128, 128], F32) for i in range(2)] for h in range(2)]
    Ptm = [[C.sb(f"Ptm{h}_{i}", [128, 128], F32) for i in range(2)] for h in range(2)]
    Tb = [C.sb(f"Tb{h}", [128, 128], BF16) for h in range(2)]
    nwT = [C.sb(f"nwT{h}", [128, 128], BF16) for h in range(2)]
    vnb = [C.sb(f"vnb{h}", [128, 256], BF16) for h in range(2)]
    dcb = [C.sb(f"dcb{h}", [128, 2], F32) for h in range(2)]
    P = [C.ps(f"P{i}") for i in range(8)]
    QA = QAlloc(P, [[5], [7]])
    xTv = xT_d.rearrange("(k p) t -> p k t", p=128)
    pj_i = [0]

    def pj_slot():
        i = pj_i[0]
        pj_i[0] = (i + 1) % 2
        return P[i][:, 0:256], f'P{i}pj'

    def proj(tt):
        t0 = tt * TT
        xnb = xn[tt % 2]
        xnk = f'xn{tt % 2}'
        par = tt % 2
        so = so2[par]
        smt, sm_tmp, dt_t, dA_t, g_t, beta_t, lnb_t = smt2[par], sm_tmp2[par], dt_t2[par], dA_t2[par], g_t2[par], beta_t2[par], lnb_t2[par]
        ys = ystage[par]
        ysk = f'ystage{par}'
        for c in range(4):
            b = c % 2
            S.dma('sp', xs[b][:], xTv[:, 4 * c:4 * c + 4, t0:t0 + TT], writes=[f'xs{b}'])
            S.op('act', lambda e, b=b: e.activation(out=xsq[b][:], in_=xs[b][:], func=AF.Square),
                 reads=[f'xs{b}'], writes=[f'xsq{b}'])
            for kk in range(4):
                k = 4 * c + kk
                S.op('pool', lambda e, b=b, kk=kk, k=k: e.tensor_scalar(
                    out=xnb[:, k, :], in0=xs[b][:, kk, :], scalar1=mixw[:, k:k + 1], scalar2=0.0,
                    op0=ALU.mult, op1=ALU.add), reads=[f'xs{b}', 'mixw'], writes=[xnk])
                S.op('pe', lambda e, b=b, kk=kk, k=k: e.matmul(P[2][:, 0:TT], lhsT=ones_b[:], rhs=xsq[b][:, kk, :],
                                                               start=(k == 0), stop=(k == 15)),
                     reads=['ones_b', f'xsq{b}'], writes=['P2a'])
        S.op('act', lambda e: e.activation(out=rb[:], in_=P[2][:, 0:TT], func=AF.Ln, scale=1.0 / D_MODEL, bias=K['eps_t'][:]),
             reads=['P2a', 'eps_t'], writes=['rb'])
        S.op('act', lambda e: e.activation(out=rb[:], in_=rb[:], func=AF.Exp, scale=-0.5), reads=['rb'], writes=['rb'])
        for blk in range(NF + 1):
            ncol = 128 if blk < NF else 16
            pj, pjk = pj_slot()
            for k in range(16):
                S.op('pe', lambda e, k=k, blk=blk, ncol=ncol, pj=pj: e.matmul(
                    pj[0:ncol, :], lhsT=W[:, k, blk * 128:blk * 128 + ncol], rhs=xnb[:, k, :],
                    start=(k == 0), stop=(k == 15)), reads=[f'W{k}', xnk], writes=[pjk])
            if blk < NF:
                S.op('dve', lambda e, blk=blk, pj=pj: e.tensor_tensor(out=convin[:, blk, 3:3 + TT], in0=pj, in1=rb[:], op=ALU.mult),
                     reads=[pjk, 'rb'], writes=[f'convin{blk}'])
            else:
                S.op('dve', lambda e, pj=pj: e.tensor_tensor(out=smf[:], in0=pj[0:16, :], in1=rb[0:16, :], op=ALU.mult),
                     reads=[pjk, 'rb'], writes=['smf'])
        for blk in range(NF):
            ca = cacc[blk % 2]
            cak = f'cacc{blk % 2}'
            S.op('dve', lambda e, blk=blk, ca=ca: e.tensor_scalar(out=ca[:], in0=convin[:, blk, 0:TT], scalar1=convw[:, blk, 0:1],
                                                                 scalar2=None, op0=ALU.mult),
                 reads=[f'convin{blk}', 'convw'], writes=[cak])
            for j in range(1, 4):
                S.op('dve', lambda e, blk=blk, ca=ca, j=j: e.scalar_tensor_tensor(
                    out=ca[:], in0=convin[:, blk, j:j + TT], scalar=convw[:, blk, j:j + 1], in1=ca[:],
                    op0=ALU.mult, op1=ALU.add), reads=[f'convin{blk}', 'convw', cak], writes=[cak])
            S.op('act', lambda e, blk=blk, ca=ca: e.activation(out=so[:, blk, :], in_=ca[:], func=AF.Silu, bias=convb[:, blk:blk + 1]),
                 reads=[cak, 'convb'], writes=[f'so{par}_{blk}'])
            S.op('pool', lambda e, blk=blk: e.tensor_copy(out=convin[:, blk, 0:3], in_=convin[:, blk, TT:TT + 3]),
                 reads=[f'convin{blk}'], writes=[f'convin{blk}'])
        for sub in range(2):
            S.op('pe', lambda e, sub=sub: e.transpose(P[2][:, 256 + 16 * sub:256 + 16 * sub + 16], smf[:, sub * 128:(sub + 1) * 128], ident_f[0:16, 0:16]),
                 reads=['smf', 'ident_f'], writes=['P2b'])
        S.op('dve', lambda e: e.tensor_copy(out=smt[:], in_=P[2][:, 256:288].rearrange("p (s c) -> p s c", s=2)),
             reads=['P2b'], writes=[f'smt{par}'])
        S.op('dve', lambda e: e.tensor_tensor(out=sm_tmp[:, :, 0:8], in0=smt[:, :, 0:8],
                                              in1=bc(hp[:, 0:8], 1, 2), op=ALU.add),
             reads=[f'smt{par}', 'hp'], writes=[f'sm_tmp{par}'])
        S.op('dve', lambda e: e.tensor_tensor(out=sm_tmp[:, :, 10:12], in0=smt[:, :, 10:12],
                                              in1=bc(hp[:, 20:22], 1, 2), op=ALU.add),
             reads=[f'smt{par}', 'hp'], writes=[f'sm_tmp{par}'])
        S.op('act', lambda e: e.activation(out=sm_tmp[:, :, 0:8], in_=sm_tmp[:, :, 0:8], func=AF.Exp), reads=[f'sm_tmp{par}'], writes=[f'sm_tmp{par}'])
        S.op('act', lambda e: e.activation(out=sm_tmp[:, :, 10:12], in_=sm_tmp[:, :, 10:12], func=AF.Exp), reads=[f'sm_tmp{par}'], writes=[f'sm_tmp{par}'])
        S.op('act', lambda e: e.activation(out=dt_t[:], in_=sm_tmp[:, :, 0:8], func=AF.Ln, bias=K['one_t'][:]), reads=[f'sm_tmp{par}', 'one_t'], writes=[f'dt_t{par}'])
        S.op('act', lambda e: e.activation(out=sm_tmp[:, :, 10:12], in_=sm_tmp[:, :, 10:12], func=AF.Ln, bias=K['one_t'][:]),
             reads=[f'sm_tmp{par}', 'one_t'], writes=[f'sm_tmp{par}'])
        S.op('dve', lambda e: e.tensor_tensor(out=dA_t[:], in0=dt_t[:], in1=bc(hq[:, 0:8], 1, 2), op=ALU.mult),
             reads=[f'dt_t{par}', 'hq'], writes=[f'dA_t{par}'])
        S.op('dve', lambda e: e.tensor_tensor(out=g_t[:], in0=sm_tmp[:, :, 10:12], in1=bc(hq[:, 8:10], 1, 2), op=ALU.mult),
             reads=[f'sm_tmp{par}', 'hq'], writes=[f'g_t{par}'])
        S.op('act', lambda e: e.activation(out=beta_t[:], in_=smt[:, :, 8:10], func=AF.Sigmoid), reads=[f'smt{par}'], writes=[f'beta_t{par}'])
        S.op('act', lambda e: e.activation(out=lnb_t[:], in_=beta_t[:], func=AF.Ln), reads=[f'beta_t{par}'], writes=[f'lnb_t{par}'])

    def ssd(tt):
        par = tt % 2
        so = so2[par]
        smt, sm_tmp, dt_t, dA_t, g_t, beta_t, lnb_t = smt2[par], sm_tmp2[par], dt_t2[par], dA_t2[par], g_t2[par], beta_t2[par], lnb_t2[par]
        ys = ystage[par]
        ysk = f'ystage{par}'
        for sub in range(2):
            cs = slice(sub * 128, (sub + 1) * 128)
            for b in range(4):
                S.op('pe', lambda e, b=b: e.transpose(P[3][:, b * 128:(b + 1) * 128], so[:, b, cs], ident_f[:]),
                     reads=[f'so{par}_{b}', 'ident_f'], writes=[f'P3q{b}'])
            S.op('act', lambda e: e.copy(out=xtok[:].rearrange("p r c -> p (r c)"), in_=P[3][:]), reads=kq(3), writes=['xtok'])
            S.op('pe', lambda e: e.transpose(P[4][:, 0:128], so[:, 4, cs], ident_f[:]), reads=[f'so{par}_4', 'ident_f'], writes=['P4q0'])
            S.op('act', lambda e: e.copy(out=btok[:], in_=P[4][:, 0:128]), reads=['P4q0'], writes=['btok'])
            S.op('dve', lambda e: e.tensor_copy(out=bfb[:], in_=so[:, 4, cs]), reads=[f'so{par}_4'], writes=['bfb'])
            S.op('dve', lambda e: e.tensor_copy(out=cfb[:], in_=so[:, 5, cs]), reads=[f'so{par}_5'], writes=['cfb'])
            S.op('pe', lambda e: e.matmul(P[6][:, 0:8], lhsT=tri_f[:], rhs=dA_t[:, sub, :], start=True, stop=True),
                 reads=['tmp_ge', f'dA_t{par}'], writes=['P6a'])
            S.op('pe', lambda e: e.matmul(P[6][:, 8:16], lhsT=ones_f[:], rhs=dA_t[:, sub, :], start=True, stop=True),
                 reads=['ones_f', f'dA_t{par}'], writes=['P6b'])
            S.op('dve', lambda e: e.tensor_copy(out=acs[:], in_=P[6][:, 0:8]), reads=['P6a'], writes=['acs'])
            S.op('dve', lambda e: e.tensor_scalar(out=nacs[:], in0=P[6][:, 0:8], scalar1=-1.0, scalar2=None, op0=ALU.mult),
                 reads=['P6a'], writes=['nacs'])
            S.op('dve', lambda e: e.tensor_tensor(out=tend[:], in0=P[6][:, 8:16], in1=acs[:], op=ALU.subtract),
                 reads=['P6b', 'acs'], writes=['tend'])
            S.op('act', lambda e: e.activation(out=tend[:], in_=tend[:], func=AF.Exp), reads=['tend'], writes=['tend'])
            S.op('act', lambda e: e.activation(out=cdec[:], in_=P[6][:, 8:16], func=AF.Exp), reads=['P6b'], writes=['cdec'])
            S.op('dve', lambda e: e.tensor_tensor(out=wsc[:], in0=dt_t[:, sub, :], in1=tend[:], op=ALU.mult),
                 reads=[f'dt_t{par}', 'tend'], writes=['wsc'])
            S.op('dve', lambda e: e.tensor_tensor(out=xdt[:], in0=xtok[:], in1=bc(dt_t[:, sub, :], 2, 64), op=ALU.mult),
                 reads=['xtok', f'dt_t{par}'], writes=['xdt'])
            S.op('dve', lambda e: e.tensor_tensor(out=xdtw[:], in0=xtok[:], in1=bc(wsc[:], 2, 64), op=ALU.mult),
                 reads=['xtok', 'wsc'], writes=['xdtw'])
            S.op('pool', lambda e: e.tensor_tensor(out=Xda[:], in0=bc(tri_f[:], 1, 8),
                                                   in1=bc(dA_t[:, sub, :], 2, 128), op=ALU.mult),
                 reads=['tmp_ge', f'dA_t{par}'], writes=['Xda'])
            for hf in range(2):
                S.op('pe', lambda e, hf=hf: e.matmul(P[4][:], lhsT=ones_f[:], rhs=Xda[:, 4 * hf:4 * hf + 4, :].rearrange("p r l -> p (r l)"),
                                                     start=True, stop=False), reads=['ones_f', 'Xda'], writes=kq(4))
                S.op('pe', lambda e, hf=hf: e.matmul(P[4][:], lhsT=ident_f[:], rhs=nmrep[:, 4 * hf:4 * hf + 4, :].rearrange("p r l -> p (r l)"),
                                                     start=False, stop=True), reads=['ident_f', 'nmrep'], writes=kq(4))
                for r in range(4 * hf, 4 * hf + 4):
                    S.op('act', lambda e, r=r: e.activation(out=decay[:, r, :], in_=P[4][:, (r % 4) * 128:(r % 4 + 1) * 128],
                                                            func=AF.Exp, bias=nacs[:, r:r + 1]),
                         reads=[f'P4q{r % 4}', 'nacs'], writes=['decay'])
            S.op('pe', lambda e: e.matmul(P[6][:, 128:256], lhsT=bfb[:], rhs=cfb[:], start=True, stop=True), reads=['bfb', 'cfb'], writes=['P6c'])
            S.op('dve', lambda e: e.tensor_tensor(out=scor[:], in0=decay[:], in1=bc(P[6][:, 128:256], 1, 8), op=ALU.mult),
                 reads=['decay', 'P6c'], writes=['scor'])
            for hf in range(2):
                S.op('pe', lambda e, hf=hf: e.matmul(P[4][:], lhsT=ones_f[:], rhs=Xda[:, 4 * hf:4 * hf + 4, :].rearrange("p r l -> p (r l)"),
                                                     start=True, stop=True), reads=['ones_f', 'Xda'], writes=kq(4))
                S.op('act', lambda e, hf=hf: e.activation(out=Eb[:, 4 * hf:4 * hf + 4, :].rearrange("p r l -> p (r l)"), in_=P[4][:], func=AF.Exp),
                     reads=kq(4), writes=['Eb'])
            S.op('pool', lambda e: e.tensor_tensor(out=Cdec[:], in0=Eb[:], in1=bc(so[:, 5, cs], 1, 8), op=ALU.mult),
                 reads=['Eb', f'so{par}_5'], writes=['Cdec'])
            for r in range(8):
                b, half = r // 2, r % 2
                out = P[3][half * 64:(half + 1) * 64, b * 128:(b + 1) * 128]
                S.op('pe', lambda e, r=r, out=out: e.matmul(out, lhsT=xdt[:, r, :], rhs=scor[:, r, :], start=True, stop=False),
                     reads=['xdt', 'scor'], writes=[f'P3q{b}'])
                S.op('pe', lambda e, r=r, out=out: e.matmul(out, lhsT=h_b[:, r * 64:(r + 1) * 64], rhs=Cdec[:, r, :], start=False, stop=True),
                     reads=['h_b', 'Cdec'], writes=[f'P3q{b}'])
            for b in range(4):
                S.op('dve', lambda e, b=b: e.scalar_tensor_tensor(out=ys[:, b, cs], in0=so[:, b, cs], scalar=hp[:, 16 + b:17 + b],
                                                                 in1=P[3][:, b * 128:(b + 1) * 128], op0=ALU.mult, op1=ALU.add),
                     reads=[f'so{par}_{b}', 'hp', f'P3q{b}'], writes=[ysk + 's'])
            S.op('pe', lambda e: e.matmul(P[6][:, :], lhsT=btok[:], rhs=xdtw[:].rearrange("p r c -> p (r c)"), start=True, stop=True),
                 reads=['btok', 'xdtw'], writes=['P6a', 'P6b', 'P6c'])
            S.op('dve', lambda e: e.tensor_tensor(out=h_f[:], in0=h_f[:], in1=bc(cdec[:], 2, 64), op=ALU.mult),
                 reads=['h_f', 'cdec'], writes=['h_f'])
            S.op('dve', lambda e: e.tensor_tensor(out=h_f[:], in0=h_f[:], in1=P[6][:].rearrange("p (r c) -> p r c", r=8), op=ALU.add),
                 reads=['h_f', 'P6a', 'P6b', 'P6c'], writes=['h_f'])
            S.op('act', lambda e: e.copy(out=h_b[:], in_=h_f[:].rearrange("p r c -> p (r c)")), reads=['h_f'], writes=['h_b'])

    def gdn(tt):
        par = tt % 2
        so = so2[par]
        smt, sm_tmp, dt_t, dA_t, g_t, beta_t, lnb_t = smt2[par], sm_tmp2[par], dt_t2[par], dA_t2[par], g_t2[par], beta_t2[par], lnb_t2[par]
        ys = ystage[par]
        ysk = f'ystage{par}'
        for sub in range(2):
            gdn_pair(C, K, P, QA, so, par, ys, ysk + 'g', sub, g_t, beta_t, lnb_t, Sg_f, Sg_b, gsq, rn, kn, qn, qd, kb, kdec, vb, gsc,
                     Xg, Xg2, Eg, Ee, Es, attnT, Mm, Lm, Tm, Ttm, Pm, Ptm, Tb, nwT, vnb, dcb)

    proj(0)
    for tt in range(NTT):
        fns, wts = [], []
        if tt + 1 < NTT:
            fns.append(lambda tt=tt: proj(tt + 1))
            wts.append(3)
        fns.append(lambda tt=tt: ssd(tt))
        wts.append(1)
        fns.append(lambda tt=tt: gdn(tt))
        wts.append(4)
        C.S.coop.run(fns, wts)
        store(tt, ystage[tt % 2], [f'ystage{tt % 2}s', f'ystage{tt % 2}g'])


def build_p1(T):
    nc = bass.Bass("TRN2", target_bir_lowering=False)
    D = p1_decl(nc, T)
    y_d = nc.dram_tensor("y1T", [1024, T], BF16, kind="ExternalOutput").ap()
    yv = y_d.rearrange("(b p) t -> p b t", p=128)
    with ExitStack() as es:
        C = Ctx(nc, es)
        K = build_consts(C)
        p1_body(C, K, T, D, lambda tt, ys, ysk: C.S.dma('sp', yv[:, :, tt * TT:(tt + 1) * TT], ys[:], reads=list(ysk), writes=['y_out']))
        C.S.barrier()
    return nc


def prep_p1(inp, g, T):
    w_in = inp["w_in"][0]
    base = SSM_PROJ
    cols = np.concatenate([
        4096 + g * 512 + np.arange(512),
        4096 + 4096 + g * 128 + np.arange(128),
        4096 + 4096 + 1024 + g * 128 + np.arange(128),
        base + 2 * g * 128 + np.arange(256),
        base + 2048 + 2 * g * 128 + np.arange(256),
        base + 4096 + 2 * g * 256 + np.arange(512),
        10240 + g * 8 + np.arange(8),
        base + 12288 + 2 * g + np.arange(2),
        base + 12288 + 16 + 2 * g + np.arange(2),
    ])
    w1 = np.zeros((D_MODEL, NCOL1), np.float32)
    w1[:, :cols.size] = w_in[:, cols]
    scw, scb, gcw = inp["ssm_conv_w"][0], inp["ssm_conv_b"][0], inp["gdn_conv_w"][0]
    cw = np.concatenate([
        scw[:, g * 512:(g + 1) * 512], scw[:, 4096 + g * 128:4096 + (g + 1) * 128],
        scw[:, 5120 + g * 128:5120 + (g + 1) * 128],
        gcw[:, 2 * g * 128:2 * g * 128 + 256], gcw[:, 2048 + 2 * g * 128:2048 + 2 * g * 128 + 256],
        gcw[:, 4096 + 2 * g * 256:4096 + 2 * g * 256 + 512]], axis=1)
    convw = np.ascontiguousarray(cw.reshape(4, NF, 128).transpose(2, 1, 0))
    cb = np.zeros(NF * 128, np.float32)
    cb[0:512] = scb[g * 512:(g + 1) * 512]
    cb[512:640] = scb[4096 + g * 128:4096 + (g + 1) * 128]
    cb[640:768] = scb[5120 + g * 128:5120 + (g + 1) * 128]
    convb = np.ascontiguousarray(cb.reshape(NF, 128).T)
    hp = np.zeros((128, 32), np.float32)
    hp[:, 0:8] = inp["ssm_dt_bias"][0][g * 8:(g + 1) * 8]
    hp[:, 8:16] = inp["ssm_a_log"][0][g * 8:(g + 1) * 8]
    dd = inp["ssm_d"][0][g * 8:(g + 1) * 8]
    for b in range(4):
        hp[0:64, 16 + b] = dd[2 * b]
        hp[64:128, 16 + b] = dd[2 * b + 1]
    hp[:, 20:22] = inp["gdn_dt_bias"][0][2 * g:2 * g + 2]
    hp[:, 22:24] = inp["gdn_a_log"][0][2 * g:2 * g + 2]
    mixw = np.ascontiguousarray(inp["mix_norm_w"][0].reshape(16, 128).T)
    return {"w1": w1, "convw": convw, "convb": convb, "hp": hp, "mixw": mixw}


def peer_group(C, K, P, PA, h1, nwb_d, wq_d, skT, u4_d, v_d, neb):
    S = C.S
    ones_b, ident_b = K['ones_b'], K['ident_b']
    C.push()
    hnT = C.sb("hnT", [128, 16, TG], BF16)
    s_sb = C.sb("s_sb", [128, 4, 16, 128], F32)
    kap = C.sb("kap", [128, 4, 8], F32)
    C.push()
    ffw = C.sb("ffw", [128, D_MODEL], F32)
    hn = C.sb("hn", [128, 4, D_MODEL], BF16)
    qT = C.sb("qT", [128, 16, TG], BF16)
    wqc = [C.sb(f"wqc{i}", [128, 16, 512], BF16) for i in range(2)]
    ss4 = C.sb("ss4", [128, 4], F32)
    junk = C.sb("junk2", [128, D_MODEL], BF16)
    tops = C.sb("tops", [128, 2, 16], F32)
    sc = C.sb("sc", [128, 128], F32)
    cand = C.sb("cand", [128, 256], F32)
    cand2 = C.sb("cand2", [128, 256], F32)
    ctop = C.sb("ctop", [128, 16], F32)
    ex16 = C.sb("ex16", [128, 16], F32)
    sm = C.sb("sm", [128, 8], F32)
    S.dma('sp', ffw[:], nwb_d[:, 0, :], writes=['ffw'])
    for ts in range(4):
        S.op('act', lambda e: e.activation(out=junk[:], in_=h1[:, ts, :], func=AF.Square, accum_out=ss4[:, ts:ts + 1]),
             reads=['h1'], writes=['junk2', 'ss4'])
    S.op('act', lambda e: e.activation(out=ss4[:], in_=ss4[:], func=AF.Ln, scale=1.0 / D_MODEL, bias=K['eps_t'][:]), reads=['ss4', 'eps_t'], writes=['ss4'])
    S.op('act', lambda e: e.activation(out=ss4[:], in_=ss4[:], func=AF.Exp, scale=-0.5), reads=['ss4'], writes=['ss4'])
    for ts in range(4):
        S.op('dve', lambda e: e.scalar_tensor_tensor(out=hn[:, ts, :], in0=h1[:, ts, :], scalar=ss4[:, ts:ts + 1], in1=ffw[:],
                                                    op0=ALU.mult, op1=ALU.mult), reads=['h1', 'ss4', 'ffw'], writes=[f'hn{ts}'])
    n = 0
    for ts in range(4):
        for kg in range(4):
            b = [0, 3][n % 2]
            n += 1
            for kk in range(4):
                k = 4 * kg + kk
                S.op('pe', lambda e: e.matmul(P[b][:, kk * 128:(kk + 1) * 128], lhsT=hn[:, ts, k * 128:(k + 1) * 128], rhs=ident_b[:], start=True, stop=True),
                     reads=[f'hn{ts}', 'ident_b'], writes=[f'P{b}t'])
            if n % 2:
                S.op('act', lambda e: e.copy(out=hnT[:, 4 * kg:4 * kg + 4, ts * 128:(ts + 1) * 128], in_=P[b][:].rearrange("p (a t) -> p a t", a=4)),
                     reads=[f'P{b}t'], writes=['hnT'])
            else:
                S.op('dve', lambda e: e.tensor_copy(out=hnT[:, 4 * kg:4 * kg + 4, ts * 128:(ts + 1) * 128], in_=P[b][:].rearrange("p (a t) -> p a t", a=4)),
                     reads=[f'P{b}t'], writes=['hnT'])
    for cg in range(4):
        S.dma('pool', wqc[cg % 2][:], wq_d[cg], writes=[f'wqc{cg % 2}'])
        for cb in range(4):
            b = 1 + (cb % 2)
            for k in range(16):
                S.op('pe', lambda e: e.matmul(P[b][:], lhsT=wqc[cg % 2][:, k, cb * 128:(cb + 1) * 128], rhs=hnT[:, k, :], start=(k == 0), stop=(k == 15)),
                     reads=[f'wqc{cg % 2}', 'hnT'], writes=[f'P{b}z'])
            S.op('act', lambda e: e.copy(out=qT[:, 4 * cg + cb, :], in_=P[b][:]), reads=[f'P{b}z'], writes=[f'qT{4 * cg + cb}'])
    n = 0
    for ts in range(4):
        for c4 in range(4):
            b = [0, 3][n % 2]
            n += 1
            for j in range(4):
                c = 4 * c4 + j
                S.op('pe', lambda e: e.matmul(P[b][:, j * 128:(j + 1) * 128], lhsT=qT[:, c, ts * 128:(ts + 1) * 128], rhs=skT[:, c, :], start=True, stop=True),
                     reads=[f'qT{c}', 'skT'], writes=[f'P{b}t'])
            S.op('dve', lambda e: e.tensor_copy(out=s_sb[:, ts, 4 * c4:4 * c4 + 4, :], in_=P[b][:].rearrange("p (a t) -> p a t", a=4)),
                 reads=[f'P{b}t'], writes=[f's_sb{ts}'])
    for ts in range(4):
        sk = f's_sb{ts}'
        for h in range(8):
            for pp in range(2):
                src = s_sb[:, ts, 2 * h + pp, :]
                S.op('dve', lambda e: e.max(out=tops[:, pp, 0:8], in_=src), reads=[sk], writes=['tops'])
                S.op('dve', lambda e: e.match_replace(out=sc[:], in_to_replace=tops[:, pp, 0:8], in_values=src, imm_value=-1e30),
                     reads=[sk, 'tops'], writes=['sc'])
                S.op('dve', lambda e: e.max(out=tops[:, pp, 8:16], in_=sc[:]), reads=['sc'], writes=['tops'])
            S.op('dve', lambda e: e.tensor_tensor(out=cand[:].rearrange("p (a b) -> p a b", a=16), in0=bc(tops[:, 0, :], 2, 16), in1=bc(tops[:, 1, :], 1, 16), op=ALU.add),
                 reads=['tops'], writes=['cand'])
            S.op('dve', lambda e: e.max(out=ctop[:, 0:8], in_=cand[:]), reads=['cand'], writes=['ctop'])
            S.op('dve', lambda e: e.match_replace(out=cand2[:], in_to_replace=ctop[:, 0:8], in_values=cand[:], imm_value=-1e30),
                 reads=['cand', 'ctop'], writes=['cand2'])
            S.op('dve', lambda e: e.max(out=ctop[:, 8:16], in_=cand2[:]), reads=['cand2'], writes=['ctop'])
            S.op('dve', lambda e: e.tensor_scalar(out=sm[:, 0:1], in0=ctop[:, 0:1], scalar1=-1.0, scalar2=None, op0=ALU.mult), reads=['ctop'], writes=['sm'])
            S.op('act', lambda e: e.activation(out=ex16[:], in_=ctop[:], func=AF.Exp, bias=sm[:, 0:1], accum_out=sm[:, 1:2]),
                 reads=['ctop', 'sm'], writes=['ex16', 'sm'])
            S.op('act', lambda e: e.activation(out=sm[:, 2:3], in_=sm[:, 1:2], func=AF.Ln), reads=['sm'], writes=['sm'])
            S.op('dve', lambda e: e.tensor_tensor(out=sm[:, 3:4], in0=sm[:, 0:1], in1=sm[:, 2:3], op=ALU.subtract), reads=['sm'], writes=['sm'])
            S.op('dve', lambda e: e.tensor_scalar(out=s_sb[:, ts, 2 * h, :], in0=s_sb[:, ts, 2 * h, :], scalar1=sm[:, 3:4], scalar2=None, op0=ALU.add),
                 reads=[sk, 'sm'], writes=[sk])
            S.op('dve', lambda e: e.tensor_scalar(out=sm[:, 4:5], in0=ctop[:, 15:16], scalar1=sm[:, 3:4], scalar2=-1e-4, op0=ALU.add, op1=ALU.add),
                 reads=['ctop', 'sm'], writes=['sm'])
            S.op('act', lambda e: e.activation(out=kap[:, ts, h:h + 1], in_=sm[:, 4:5], func=AF.Exp), reads=['sm'], writes=['kap'])
    C.pop()
    C.push()
    uT = [C.sb(f"uT{i}", [128, 16, 128], BF16) for i in range(3)]
    vt = [C.sb(f"vt{i}", [128, D_MODEL], BF16) for i in range(2 * GRP)]
    gS = [C.sb(f"gS{i}", [128, TG], F32) for i in range(2)]
    Eb = [C.sb(f"Eb{i}", [128, 8, 128], F32) for i in range(2)]
    Gb = [C.sb(f"Gb{i}", [128, 8, 128], BF16) for i in range(2)]
    AT = [C.sb(f"AT{i}", [128, GRP, TG], BF16) for i in range(2)]
    e2 = 0
    for grp in range(neb // GRP):
        a2 = grp % 2
        for gi_ in range(GRP):
            i = grp * GRP + gi_
            u_, uk = uT[i % 3], f'uT{i % 3}'
            v_, vk = vt[i % (2 * GRP)], f'vt{i % (2 * GRP)}'
            S.dma('pool', u_[:], u4_d[i], writes=[uk])
            S.dma('pool', v_[:], v_d[i * 128:(i + 1) * 128, :], writes=[vk])
            sb_ = 1 + (i % 2)
            for k in range(16):
                S.op('pe', lambda e: e.matmul(P[sb_][:], lhsT=u_[:, k, :], rhs=hnT[:, k, :], start=(k == 0), stop=(k == 15)),
                     reads=[uk, 'hnT'], writes=[f'P{sb_}z'])
            S.op('act', lambda e: e.activation(out=gS[i % 2][:], in_=P[sb_][:], func=AF.Gelu), reads=[f'P{sb_}z'], writes=[f'gS{i % 2}'])
            gb_ = [0, 3][i % 2]
            for ts in range(4):
                for h in range(8):
                    S.op('act', lambda e: e.activation(out=Eb[e2][:, h, :], in_=s_sb[:, ts, 2 * h + 1, :], func=AF.Exp, bias=s_sb[:, ts, 2 * h, i:i + 1]),
                         reads=[f's_sb{ts}'], writes=[f'Eb{e2}'])
                for h in range(8):
                    S.op('dve', lambda e: e.scalar_tensor_tensor(out=Gb[e2][:, h, :], in0=Eb[e2][:, h, :], scalar=kap[:, ts, h:h + 1], in1=Eb[e2][:, h, :],
                                                                op0=ALU.is_ge, op1=ALU.mult), reads=[f'Eb{e2}', 'kap'], writes=[f'Gb{e2}'])
                for h in range(8):
                    S.op('pe', lambda e: e.matmul(P[gb_][:, ts * 128:(ts + 1) * 128], lhsT=Gb[e2][:, h, :], rhs=ident_b[:], start=(h == 0), stop=(h == 7)),
                         reads=[f'Gb{e2}', 'ident_b'], writes=[f'P{gb_}t'])
                e2 = 1 - e2
            S.op('dve', lambda e: e.tensor_tensor(out=AT[a2][:, gi_, :], in0=P[gb_][:], in1=gS[i % 2][:], op=ALU.mult),
                 reads=[f'P{gb_}t', f'gS{i % 2}'], writes=[f'AT{a2}'])
        for ts in range(4):
            for dc in range(4):
                for gi_ in range(GRP):
                    i = grp * GRP + gi_
                    S.op('pe', lambda e: e.matmul(PA[:, dc * 512:(dc + 1) * 512], lhsT=AT[a2][:, gi_, ts * 128:(ts + 1) * 128],
                                                  rhs=vt[i % (2 * GRP)][:, dc * 512:(dc + 1) * 512], start=(gi_ == 0), stop=(gi_ == GRP - 1)),
                         reads=[f'AT{a2}', f'vt{i % (2 * GRP)}'], writes=[f'P{4 + dc}acc'])
            S.op('dve', lambda e: e.tensor_tensor(out=h1[:, ts, :], in0=PA[:], in1=h1[:, ts, :], op=ALU.add),
                 reads=[f'P{4 + d}acc' for d in range(4)] + ['h1'], writes=['h1'])
    C.pop()
    C.pop()

TG = 512
GRP = 4
NEB = 128


def tile_w(Wm, chunk):
    Kd, N = Wm.shape
    return np.ascontiguousarray(Wm.reshape(Kd // 128, 128, N // chunk, chunk).transpose(2, 1, 0, 3))


def p2_decl(nc, TL, xtn="xT"):
    return dict(
        xT=nc.dram_tensor(xtn, [D_MODEL, TL], F32, kind="ExternalInput").ap(),
        xtok=nc.dram_tensor("xtok", [TL, D_MODEL], F32, kind="ExternalInput").ap(),
        wzg=nc.dram_tensor("wzg", [32, 128, 16, 256], F32, kind="ExternalInput").ap(),
        wgt=nc.dram_tensor("wgt", [32, 128, 16, 128], F32, kind="ExternalInput").ap(),
        wbs=nc.dram_tensor("wbs", [16, 128, 32, 128], F32, kind="ExternalInput").ap(),
        wbg=nc.dram_tensor("wbg", [16, 128, 32, 128], F32, kind="ExternalInput").ap(),
        wout=nc.dram_tensor("wout", [8, 128, 16, 256], F32, kind="ExternalInput").ap(),
        wq=nc.dram_tensor("wq", [4, 128, 16, 512], F32, kind="ExternalInput").ap(),
        skT=nc.dram_tensor("skT", [128, 16, 128], F32, kind="ExternalInput").ap(),
        u4=nc.dram_tensor("u4", [NEB, 128, 16, 128], F32, kind="ExternalInput").ap(),
        v=nc.dram_tensor("vtab", [NEB * 128, D_MODEL], F32, kind="ExternalInput").ap(),
        sp=nc.dram_tensor("sp2", [128, 128], F32, kind="ExternalInput").ap(),
        nwb=nc.dram_tensor("nwb", [128, 2, D_MODEL], F32, kind="ExternalInput").ap(),
        out=nc.dram_tensor("out", [TL, D_MODEL], F32, kind="ExternalOutput").ap(),)


def p2_body(C, K, TL, D, yload, neb=NEB):
    S = C.S
    NG = TL // TG
    xT_d = D['xT']
    xtok_d = D['xtok']
    wzg_d = D['wzg']
    wgt_d = D['wgt']
    wbs_d = D['wbs']
    wbg_d = D['wbg']
    wout_d = D['wout']
    wq_d = D['wq']
    skT_d = D['skT']
    u4_d = D['u4']
    v_d = D['v']
    sp_d = D['sp']
    nwb_d = D['nwb']
    out_d = D['out']
    ones_b, ident_b = K['ones_b'], K['ident_b']
    P = [C.ps(f"P{i}") for i in range(4)]
    PA = C.ps("PA", [128, 2048], F32)
    spm = C.sb("spm", [128, 128], F32)
    S.dma('sp', spm[:], sp_d, writes=['spm'])
    skT = C.sb("skT_s", [128, 16, 128], BF16)
    S.dma('pool', skT[:], skT_d, writes=['skT'])
    h1 = C.sb("h1", [128, 4, D_MODEL], F32)
    xTv = xT_d.rearrange("(k p) t -> p k t", p=128)
    xtv = xtok_d.rearrange("(s p) d -> p s d", p=128)
    outv = out_d.rearrange("(s p) d -> p s d", p=128)
    pi = [0]

    def pbank():
        i = pi[0]
        pi[0] = (i + 1) % 2
        return P[1 + i], f'P{1 + i}z'

    def rsq(out, in_, scale, reads, writes):
        S.op('act', lambda e: e.activation(out=out, in_=in_, func=AF.Ln, scale=scale, bias=K['eps_t'][:]), reads=reads + ['eps_t'], writes=writes)
        S.op('act', lambda e: e.activation(out=out, in_=out, func=AF.Exp, scale=-0.5), reads=writes, writes=writes)

    for gi in range(NG):
        tg0 = gi * TG
        tsl = slice(tg0, tg0 + TG)
        S.dma('sp', h1[:], xtv[:, 4 * gi:4 * gi + 4, :], writes=['h1'])
        C.push()
        xn = C.sb("xn", [128, 16, TG], BF16)
        rb = C.sb("rb", [128, TG], F32)
        Yn = C.sb("Yn", [128, 64, TG], BF16)
        C.push()
        xs = [C.sb(f"xs{i}", [128, 4, TG], F32) for i in range(2)]
        xsq = [C.sb(f"xsq{i}", [128, 4, TG], BF16) for i in range(2)]
        Wzc = [C.sb(f"Wzc{i}", [128, 16, 256], BF16) for i in range(2)]
        Yr = [C.sb(f"Yr{i}", [128, 4, TG], BF16) for i in range(2)]
        zt = [C.sb(f"zt{i}", [128, TG], F32) for i in range(2)]
        yg = C.sb("yg", [128, 4, TG], F32)
        sqb = [C.sb(f"sqb{i}", [128, TG], BF16) for i in range(2)]
        rgb = C.sb("rgb", [128, TG], F32)
        for c in range(4):
            b = c % 2
            S.dma('sp', xs[b][:], xTv[:, 4 * c:4 * c + 4, tsl], writes=[f'xs{b}'])
            S.op('act', lambda e: e.activation(out=xsq[b][:], in_=xs[b][:], func=AF.Square), reads=[f'xs{b}'], writes=[f'xsq{b}'])
            for kk in range(4):
                k = 4 * c + kk
                S.op('pool', lambda e: e.tensor_scalar(out=xn[:, k, :], in0=xs[b][:, kk, :], scalar1=spm[:, k:k + 1], scalar2=0.0,
                                                       op0=ALU.mult, op1=ALU.add), reads=[f'xs{b}', 'spm'], writes=['xn'])
                S.op('pe', lambda e: e.matmul(P[0][:], lhsT=ones_b[:], rhs=xsq[b][:, kk, :], start=(k == 0), stop=(k == 15)),
                     reads=['ones_b', f'xsq{b}'], writes=['P0ss'])
        rsq(rb[:], P[0][:], 1.0 / D_MODEL, ['P0ss'], ['rb'])
        wi = [0]

        def load_wz(chunk):
            i = wi[0]
            wi[0] = (i + 1) % 2
            S.dma('pool', Wzc[i][:], wzg_d[chunk], writes=[f'Wzc{i}'])
            return Wzc[i], f'Wzc{i}'

        def zproj(Wt, Wk, cb):
            pb, pk = pbank()
            for k in range(16):
                S.op('pe', lambda e: e.matmul(pb[:], lhsT=Wt[:, k, cb * 128:(cb + 1) * 128], rhs=xn[:, k, :], start=(k == 0), stop=(k == 15)),
                     reads=[Wk, 'xn'], writes=[pk])
            j = zproj.i
            zproj.i = (j + 1) % 2
            S.op('dve', lambda e: e.tensor_tensor(out=zt[j][:], in0=pb[:], in1=rb[:], op=ALU.mult), reads=[pk, 'rb'], writes=[f'zt{j}'])
            S.op('act', lambda e: e.activation(out=zt[j][:], in_=zt[j][:], func=AF.Silu), reads=[f'zt{j}'], writes=[f'zt{j}'])
            return zt[j], f'zt{j}'
        zproj.i = 0
        for gg in range(8):
            yb = gg % 2
            S.dma('sp', Yr[yb][:], yload(4 * gg, 4 * gg + 4, tsl), reads=['yown'], writes=[f'Yr{yb}'])
            for b in range(4):
                if b % 2 == 0:
                    Wt, Wk = load_wz(2 * gg + b // 2)
                z_, zk = zproj(Wt, Wk, b % 2)
                S.op('dve', lambda e: e.tensor_tensor(out=yg[:, b, :], in0=Yr[yb][:, b, :], in1=z_[:], op=ALU.mult),
                     reads=[f'Yr{yb}', zk], writes=[f'yg{b}'])
                S.op('act', lambda e: e.activation(out=sqb[b % 2][:], in_=yg[:, b, :], func=AF.Square), reads=[f'yg{b}'], writes=[f'sqb{b % 2}'])
                S.op('pe', lambda e: e.matmul(P[3][:], lhsT=ones_b[:], rhs=sqb[b % 2][:], start=(b == 0), stop=(b == 3)),
                     reads=['ones_b', f'sqb{b % 2}'], writes=['P3ss'])
            rsq(rgb[:], P[3][:], 1.0 / 512, ['P3ss'], ['rgb'])
            for b in range(4):
                blk = 4 * gg + b
                S.op('dve', lambda e: e.scalar_tensor_tensor(out=Yn[:, blk, :], in0=yg[:, b, :], scalar=spm[:, 48 + blk:49 + blk], in1=rgb[:],
                                                            op0=ALU.mult, op1=ALU.mult), reads=[f'yg{b}', 'spm', 'rgb'], writes=[f'Yn{blk}'])
        for hh in range(16):
            yb = (hh // 2) % 2
            o2 = 2 * (hh % 2)
            if hh % 2 == 0:
                S.dma('sp', Yr[yb][:], yload(32 + 2 * hh, 32 + 2 * hh + 4, tsl), reads=['yown'], writes=[f'Yr{yb}'])
            Wt, Wk = load_wz(16 + hh)
            for b in range(2):
                S.op('act', lambda e: e.activation(out=sqb[b][:], in_=Yr[yb][:, o2 + b, :], func=AF.Square), reads=[f'Yr{yb}'], writes=[f'sqb{b}'])
                S.op('pe', lambda e: e.matmul(P[3][:], lhsT=ones_b[:], rhs=sqb[b][:], start=(b == 0), stop=(b == 1)),
                     reads=['ones_b', f'sqb{b}'], writes=['P3ss'])
            rsq(rgb[:], P[3][:], 1.0 / 256, ['P3ss'], ['rgb'])
            for b in range(2):
                blk = 32 + 2 * hh + b
                z_, zk = zproj(Wt, Wk, b)
                S.op('dve', lambda e: e.scalar_tensor_tensor(out=yg[:, b, :], in0=Yr[yb][:, o2 + b, :], scalar=spm[:, 80 + b:81 + b], in1=rgb[:],
                                                            op0=ALU.mult, op1=ALU.mult), reads=[f'Yr{yb}', 'spm', 'rgb'], writes=[f'yg{b}'])
                S.op('pool', lambda e: e.tensor_tensor(out=Yn[:, blk, :], in0=yg[:, b, :], in1=z_[:], op=ALU.mult),
                     reads=[f'yg{b}', zk], writes=[f'Yn{blk}'])
        C.pop()
        C.push()
        Wg = [C.sb(f"Wg{i}", [128, 16, 128], BF16) for i in range(2)]
        Wbs = [C.sb(f"Wbs{i}", [128, 32, 128], BF16) for i in range(2)]
        Wbg = [C.sb(f"Wbg{i}", [128, 32, 128], BF16) for i in range(2)]
        gte = [C.sb(f"gte{i}", [128, TG], F32) for i in range(2)]
        tm = [C.sb(f"tm{i}", [128, TG], F32) for i in range(2)]
        mixb = C.sb("mixb", [128, 16, TG], BF16)
        woc = [C.sb(f"woc{i}", [128, 16, 256], BF16) for i in range(2)]
        for mb in range(16):
            for br in range(2):
                S.dma('pool', Wg[br][:], wgt_d[br * 16 + mb], writes=[f'Wg{br}'])
            i2 = mb % 2
            S.dma('pool', Wbs[i2][:], wbs_d[mb], writes=[f'Wbs{i2}'])
            S.dma('pool', Wbg[i2][:], wbg_d[mb], writes=[f'Wbg{i2}'])
            for br in range(2):
                pb, pk = pbank()
                for k in range(16):
                    S.op('pe', lambda e: e.matmul(pb[:], lhsT=Wg[br][:, k, :], rhs=xn[:, k, :],
                                                  start=(k == 0), stop=(k == 15)), reads=[f'Wg{br}', 'xn'], writes=[pk])
                S.op('dve', lambda e: e.tensor_tensor(out=gte[br][:], in0=pb[:], in1=rb[:], op=ALU.mult), reads=[pk, 'rb'], writes=[f'gte{br}'])
                S.op('act', lambda e: e.activation(out=gte[br][:], in_=gte[br][:], func=AF.Sigmoid, bias=spm[:, 16 + br * 16 + mb:17 + br * 16 + mb]),
                     reads=[f'gte{br}', 'spm'], writes=[f'gte{br}'])
            for br, Wb_ in ((0, Wbs), (1, Wbg)):
                pb, pk = pbank()
                for cb in range(32):
                    S.op('pe', lambda e: e.matmul(pb[:], lhsT=Wb_[i2][:, cb, :], rhs=Yn[:, 32 * br + cb, :], start=(cb == 0), stop=(cb == 31)),
                         reads=[f'Wb{"sg"[br]}{i2}', f'Yn{32 * br + cb}'], writes=[pk])
                S.op('dve', lambda e: e.tensor_tensor(out=tm[br][:], in0=pb[:], in1=gte[br][:], op=ALU.mult), reads=[pk, f'gte{br}'], writes=[f'tm{br}'])
            S.op('pool', lambda e: e.tensor_tensor(out=mixb[:, mb, :], in0=tm[0][:], in1=tm[1][:], op=ALU.add), reads=['tm0', 'tm1'], writes=['mixb'])
        for dc in range(8):
            S.dma('pool', woc[dc % 2][:], wout_d[dc], writes=[f'woc{dc % 2}'])
            for ts in range(4):
                pb, pk = pbank()
                for mb in range(16):
                    S.op('pe', lambda e: e.matmul(pb[:, 0:256], lhsT=mixb[:, mb, ts * 128:(ts + 1) * 128], rhs=woc[dc % 2][:, mb, :],
                                                  start=(mb == 0), stop=(mb == 15)), reads=['mixb', f'woc{dc % 2}'], writes=[pk])
                S.op('dve', lambda e: e.tensor_tensor(out=h1[:, ts, dc * 256:(dc + 1) * 256], in0=pb[:, 0:256], in1=h1[:, ts, dc * 256:(dc + 1) * 256], op=ALU.add),
                     reads=[pk, 'h1'], writes=['h1'])
        C.pop()
        C.pop()
        peer_group(C, K, P, PA, h1, nwb_d, wq_d, skT, u4_d, v_d, neb)
        C.push()
        fnw = C.sb("fnw", [128, D_MODEL], F32)
        ssf = C.sb("ssf", [128, 4], F32)
        junk = C.sb("junk", [128, D_MODEL], BF16)
        S.dma('sp', fnw[:], nwb_d[:, 1, :], writes=['fnw'])
        for ts in range(4):
            S.op('act', lambda e: e.activation(out=junk[:], in_=h1[:, ts, :], func=AF.Square, accum_out=ssf[:, ts:ts + 1]),
                 reads=['h1'], writes=['junk', 'ssf'])
        rsq(ssf[:], ssf[:], 1.0 / D_MODEL, ['ssf'], ['ssf'])
        for ts in range(4):
            S.op('dve', lambda e: e.scalar_tensor_tensor(out=h1[:, ts, :], in0=h1[:, ts, :], scalar=ssf[:, ts:ts + 1], in1=fnw[:],
                                                        op0=ALU.mult, op1=ALU.mult), reads=['h1', 'ssf', 'fnw'], writes=['h1'])
        S.dma('sp', outv[:, 4 * gi:4 * gi + 4, :], h1[:], reads=['h1'], writes=['out'])
        C.pop()


def build_p2(TL, neb=NEB):
    nc = bass.Bass("TRN2", target_bir_lowering=False)
    YT_d = nc.dram_tensor("YT", [8192, TL], BF16, kind="ExternalInput").ap()
    YTv = YT_d.rearrange("(b p) t -> p b t", p=128)
    D = p2_decl(nc, TL)
    with ExitStack() as es:
        C = Ctx(nc, es)
        K = build_consts(C)
        p2_body(C, K, TL, D, lambda a, b, tsl: YTv[:, a:b, tsl], neb)
        C.S.barrier()
    return nc


def prep_p2_shared(inp):
    w_in = inp["w_in"][0]
    base = SSM_PROJ
    wz = np.concatenate([w_in[:, 0:4096], w_in[:, base + 8192:base + 8192 + 4096]], axis=1)
    sp2 = np.zeros((128, 128), np.float32)
    sp2[:, 0:16] = inp["mix_norm_w"][0].reshape(16, 128).T
    sp2[:, 16:48] = inp["gate_b"][0].reshape(2, 16, 128).transpose(2, 0, 1).reshape(128, 32)
    sp2[:, 48:80] = inp["ssm_norm_w"][0].reshape(32, 128).T
    sp2[:, 80:82] = inp["gdn_norm_w"][0].reshape(2, 128).T
    nwb = np.empty((128, 2, D_MODEL), np.float32)
    nwb[:, 0, :] = inp["ffn_norm_w"][0]
    nwb[:, 1, :] = inp["final_norm_w"]
    return {
        "wzg": tile_w(wz, 256),
        "wgt": tile_w(w_in[:, 22624:26720], 128),
        "wbs": tile_w(inp["w_branch_ssm"][0], 128),
        "wbg": tile_w(inp["w_branch_gdn"][0], 128),
        "wout": tile_w(inp["w_out"][0], 256),
        "wq": tile_w(inp["peer_w_q"][0], 512),
        "skT": np.ascontiguousarray(inp["peer_sub_keys"][0].reshape(16, 128, 128).transpose(2, 0, 1)),
        "u4": np.ascontiguousarray(inp["peer_u"][0].reshape(128, 128, 16, 128).transpose(0, 3, 2, 1)),
        "vtab": np.ascontiguousarray(inp["peer_v"][0]),
        "sp2": sp2,
        "nwb": nwb,
    }


def build_fused(T, neb=NEB):
    TL = T // NCORES
    nc = bass.Bass("TRN2", target_bir_lowering=False)
    D1 = p1_decl(nc, T)
    D2 = p2_decl(nc, TL, xtn="xTo")
    NGl = TL // TG
    ysend = [[nc.dram_tensor(f"ysend{j}_{h}", [NGl, 512, TG], BF16) for h in range(2)] for j in range(NCORES)]
    yrecv = nc.dram_tensor("yrecv", [NCORES, 2, NCORES, NGl, 512, TG], BF16)
    yown = nc.dram_tensor("yown", [2, NCORES, NGl, 512, TG], BF16)
    with ExitStack() as es:
        C = Ctx(nc, es)
        S = C.S
        K = build_consts(C)
        csem = es.enter_context(nc.semaphore("csem"))
        ccount = [0]
        tpd = TL // TT

        def store(tt, ys, ysk):
            j, off = tt // tpd, (tt % tpd) * TT
            for h in range(2):
                dst = ysend[j][h].ap()[off // TG].rearrange("(b p) t -> p b t", p=128)[:, :, off % TG:off % TG + TT]
                S.dma('sp', dst, ys[:, 4 * h:4 * h + 4, :], reads=list(ysk), writes=[f'ysd{j}_{h}'])
            if tt % tpd == tpd - 1:
                if ccount[0] > 0:
                    S._wait('pool', (csem, ccount[0], ('cc', 0)))
                for h in range(2):
                    S._deps('pool', [f'ysd{j}_{h}'], [f'yrc{j}_{h}'])
                    ins = nc.gpsimd.collective_compute("AllGather", ALU.bypass, replica_groups=[list(range(NCORES))],
                                                       ins=[ysend[j][h].ap().rearrange("g c t -> (g c) t").opt()],
                                                       outs=[yrecv.ap()[j, h].rearrange("r g c t -> (r g c) t").opt()])
                    ccount[0] += 1
                    ins.then_inc(csem, 1)
                    S._record((csem, ccount[0], ('cc', 0)), [f'ysd{j}_{h}'], [f'yrc{j}_{h}'])

        C.push()
        p1_body(C, K, T, D1, store)
        C.pop()
        for eng in ['sp', 'pool', 'act']:
            S._wait(eng, (csem, ccount[0], ('cc', 0)))
        pid = nc.sync.partition_id()
        src = yrecv.ap().rearrange("j h r g c t -> j (h r g c t)").rearrange("j (a n) -> j a n", a=16)[bass.ds(pid, 1)]
        S.dma('sp', yown.ap().rearrange("h r g c t -> (h r g c t)").rearrange("(a n) -> a n", a=16),
              src.rearrange("o a n -> (o a) n"), writes=['yown'])

        def yload(a, b, tsl):
            h = 0 if a < 32 else 1
            r = (a - 32 * h) // 4
            assert b - a == 4 and (a - 32 * h) % 4 == 0
            return yown.ap()[h, r, tsl.start // TG].rearrange("(b p) t -> p b t", p=128)

        p2_body(C, K, TL, D2, yload, neb)
        S.barrier()
    return nc


def fused_maps(inp, T):
    TL = T // NCORES
    x = inp["x"][0][:T]
    xT = np.ascontiguousarray(x.T)
    shared = prep_p2_shared(inp)
    maps = []
    for g in range(NCORES):
        ts = slice(g * TL, (g + 1) * TL)
        m = dict(shared)
        m.update(prep_p1(inp, g, T))
        m["xT"] = xT
        m["xTo"] = np.ascontiguousarray(xT[:, ts])
        m["xtok"] = np.ascontiguousarray(x[ts])
        maps.append(m)
    return maps


_CACHE = {}


def _get(name, fn):
    if name not in _CACHE:
        _CACHE[name] = fn()
    return _CACHE[name]


def kernel(**inp):
    inp = {k: np.asarray(v) for k, v in inp.items()}
    T = SEQ
    nc = _get(("fused", T), lambda: build_fused(T))
    maps = fused_maps(inp, T)
    r = run_bass_kernel_spmd(nc, maps, core_ids=list(range(NCORES)))
    out = np.concatenate([np.asarray(r.results[j]["out"]) for j in range(NCORES)], axis=0)
    return out.reshape(1, T, D_MODEL).astype(np.float32)
```
